# Optimizing a Trainium2 kernel written in Bass

```python
import math
import jax, jax.numpy as jnp
from jax import lax
import numpy as np

D_MODEL = 2048
BATCH = 8
SEQ = 4096
DEPTH = 1

MIX_WIDTH = D_MODEL
ATTN_WIDTH = MIX_WIDTH // 2
CONV_CHANNELS = MIX_WIDTH - ATTN_WIDTH
N_ATTN_HEADS = 8
V_HEAD_DIM = ATTN_WIDTH // N_ATTN_HEADS
QK_HEAD_DIM = V_HEAD_DIM // 2
IN_COLS = 3 * ATTN_WIDTH + 2 * CONV_CHANNELS
CONV_TAPS = 31
Q_BLOCK = 128
N_GROUPS = 4
EXPERTS_PER_GROUP = 8
N_EXPERTS = N_GROUPS * EXPERTS_PER_GROUP
TOP_K = 2
D_EXPERT = D_MODEL // 2
MOE_BLOCK = 128
EPS = 1e-6
NEG_INF = -1e30

kernel_name = 'hybrid_diffattn_conformer_hmoe'


def rmsnorm(x, g):
    xf = x.astype(jnp.float32)
    y = xf * lax.rsqrt(jnp.mean(xf * xf, axis=-1, keepdims=True) + EPS)
    return (y * g.astype(jnp.float32)).astype(x.dtype)


def layernorm(x, g, b):
    xf = x.astype(jnp.float32)
    xc = xf - jnp.mean(xf, axis=-1, keepdims=True)
    var = jnp.mean(xc * xc, axis=-1, keepdims=True)
    y = xc * lax.rsqrt(var + EPS) * g.astype(jnp.float32) + b.astype(jnp.float32)
    return y.astype(x.dtype)


def lambda_init(layer):
    return 0.8 - 0.6 * math.exp(-0.3 * layer)


def alibi_slopes(n_heads):
    return 2.0 ** (-8.0 * (jnp.arange(n_heads, dtype=jnp.float32) + 1.0) / n_heads)


def diff_attention(q, k, v, lam, lam0, subln_g):
    B, S, H = q.shape[0], q.shape[1], q.shape[2]
    n_blocks = S // Q_BLOCK
    scale = QK_HEAD_DIM ** -0.5
    slopes = alibi_slopes(H)
    kpos = jnp.arange(S)
    qb = q.reshape(B, n_blocks, Q_BLOCK, H, 2, QK_HEAD_DIM).transpose(1, 0, 2, 3, 4, 5)

    def one_block(args):
        q_blk, i = args
        qpos = i * Q_BLOCK + jnp.arange(Q_BLOCK)
        s = jnp.einsum('bqhcd,bkhcd->bhcqk', q_blk, k,
                       preferred_element_type=jnp.float32) * scale
        dist = (qpos[:, None] - kpos[None, :]).astype(jnp.float32)
        s = s - slopes[None, :, None, None, None] * dist
        s = jnp.where(dist >= 0.0, s, NEG_INF)
        p = jax.nn.softmax(s, axis=-1)
        w = p[:, :, 0] - lam * p[:, :, 1]
        return jnp.einsum('bhqk,bkhd->bqhd', w.astype(v.dtype), v)

    ob = lax.map(one_block, (qb, jnp.arange(n_blocks)))
    o = ob.transpose(1, 0, 2, 3, 4).reshape(B, S, H, V_HEAD_DIM)
    o = rmsnorm(o, subln_g) * (1.0 - lam0)
    return o.reshape(B, S, H * V_HEAD_DIM)


def conformer_conv(a, gate, conv_w, conv_b, ln_g, ln_b):
    u = a * jax.nn.sigmoid(gate)
    c = lax.conv_general_dilated(u, conv_w.astype(u.dtype), window_strides=(1,),
                                 padding=[(CONV_TAPS - 1, 0)],
                                 dimension_numbers=('NWC', 'WIO', 'NWC'),
                                 feature_group_count=CONV_CHANNELS)
    c = c + conv_b
    return jax.nn.silu(layernorm(c, ln_g, ln_b))


def hier_moe(xn, w_group_router, b_group_router, w_expert_router, b_expert_router,
             w_gate, w_up, w_down):
    B, S, D = xn.shape
    T = B * S
    xf = xn.reshape(T, D)
    g_logits = (xf @ w_group_router).astype(jnp.float32) + b_group_router.astype(jnp.float32)
    g_prob = jax.nn.softmax(g_logits, axis=-1)
    g_sel = jnp.argmax(g_prob, axis=-1)
    g_w = jnp.take_along_axis(g_prob, g_sel[:, None], axis=-1)[:, 0]
    e_logits = ((xf @ w_expert_router).astype(jnp.float32)
                + b_expert_router.astype(jnp.float32)).reshape(T, N_GROUPS, EXPERTS_PER_GROUP)
    e_in_group = jnp.take_along_axis(e_logits, g_sel[:, None, None], axis=1)[:, 0]
    top_val, top_loc = lax.top_k(e_in_group, TOP_K)
    e_w = jax.nn.softmax(top_val, axis=-1) * g_w[:, None]
    e_id = g_sel[:, None] * EXPERTS_PER_GROUP + top_loc

    A = T * TOP_K
    flat_e = e_id.reshape(A)
    flat_tok = jnp.repeat(jnp.arange(T, dtype=jnp.int32), TOP_K)
    flat_w = e_w.reshape(A)
    order = jnp.argsort(flat_e)
    s_e, s_tok, s_w = flat_e[order], flat_tok[order], flat_w[order]
    counts = jnp.bincount(flat_e, length=N_EXPERTS)
    padded = (counts + MOE_BLOCK - 1) // MOE_BLOCK * MOE_BLOCK
    starts = jnp.cumsum(counts) - counts
    ends_p = jnp.cumsum(padded)
    starts_p = ends_p - padded
    dest = starts_p[s_e] + jnp.arange(A) - starts[s_e]
    n_blocks = -(-(A + N_EXPERTS * (MOE_BLOCK - 1)) // MOE_BLOCK)
    P = n_blocks * MOE_BLOCK
    pad_tok = jnp.zeros((P,), jnp.int32).at[dest].set(s_tok)
    pad_w = jnp.zeros((P,), jnp.float32).at[dest].set(s_w)
    blk_e = jnp.minimum(jnp.searchsorted(ends_p, jnp.arange(n_blocks) * MOE_BLOCK, side='right'),
                        N_EXPERTS - 1)

    def expert_block(args):
        tok, w, e = args
        rows = xf[tok]
        h = jax.nn.silu(rows @ w_gate[e]) * (rows @ w_up[e])
        return (h @ w_down[e]) * w[:, None].astype(rows.dtype)

    ys = lax.map(expert_block, (pad_tok.reshape(n_blocks, MOE_BLOCK),
                                pad_w.reshape(n_blocks, MOE_BLOCK), blk_e))
    out = jnp.zeros((T, D), xf.dtype).at[pad_tok].add(ys.reshape(P, D))
    return out.reshape(B, S, D)


def setup_inputs(seed: int = 0) -> dict:
    key = jax.random.key(seed)
    ks = jax.random.split(key, 22)
    L = DEPTH

    def nrm(k, shape, scale):
        return jax.random.normal(k, shape, jnp.float32) * scale

    return {
        'x': nrm(ks[0], (BATCH, SEQ, D_MODEL), 1.0),
        'norm_mix_g': 1.0 + nrm(ks[1], (L, D_MODEL), 0.02),
        'w_in': nrm(ks[2], (L, D_MODEL, IN_COLS), D_MODEL ** -0.5),
        'lambda_q1': nrm(ks[3], (L, QK_HEAD_DIM), 0.1),
        'lambda_k1': nrm(ks[4], (L, QK_HEAD_DIM), 0.1),
        'lambda_q2': nrm(ks[5], (L, QK_HEAD_DIM), 0.1),
        'lambda_k2': nrm(ks[6], (L, QK_HEAD_DIM), 0.1),
        'subln_g': 1.0 + nrm(ks[7], (L, V_HEAD_DIM), 0.02),
        'conv_w': nrm(ks[8], (L, CONV_TAPS, 1, CONV_CHANNELS), CONV_TAPS ** -0.5),
        'conv_b': nrm(ks[9], (L, CONV_CHANNELS), 0.02),
        'conv_ln_g': 1.0 + nrm(ks[10], (L, CONV_CHANNELS), 0.02),
        'conv_ln_b': nrm(ks[11], (L, CONV_CHANNELS), 0.02),
        'w_out': nrm(ks[12], (L, MIX_WIDTH, D_MODEL), MIX_WIDTH ** -0.5),
        'norm_ffn_g': 1.0 + nrm(ks[13], (L, D_MODEL), 0.02),
        'w_group_router': nrm(ks[14], (L, D_MODEL, N_GROUPS), D_MODEL ** -0.5),
        'b_group_router': nrm(ks[15], (L, N_GROUPS), 0.01),
        'w_expert_router': nrm(ks[16], (L, D_MODEL, N_EXPERTS), D_MODEL ** -0.5),
        'b_expert_router': nrm(ks[17], (L, N_EXPERTS), 0.01),
        'w_gate': nrm(ks[18], (L, N_EXPERTS, D_MODEL, D_EXPERT), D_MODEL ** -0.5),
        'w_up': nrm(ks[19], (L, N_EXPERTS, D_MODEL, D_EXPERT), D_MODEL ** -0.5),
        'w_down': nrm(ks[20], (L, N_EXPERTS, D_EXPERT, D_MODEL), D_EXPERT ** -0.5),
        'norm_final_g': 1.0 + nrm(ks[21], (D_MODEL,), 0.02),
    }


def reference(x, norm_mix_g, w_in, lambda_q1, lambda_k1, lambda_q2, lambda_k2, subln_g,
              conv_w, conv_b, conv_ln_g, conv_ln_b, w_out, norm_ffn_g, w_group_router,
              b_group_router, w_expert_router, b_expert_router, w_gate, w_up, w_down,
              norm_final_g):
    B, S, _ = x.shape
    h = x
    for l in range(DEPTH):
        lam0 = lambda_init(l)
        lam = (jnp.exp(jnp.sum(lambda_q1[l] * lambda_k1[l]).astype(jnp.float32))
               - jnp.exp(jnp.sum(lambda_q2[l] * lambda_k2[l]).astype(jnp.float32)) + lam0)
        xn = rmsnorm(h, norm_mix_g[l])
        proj = xn @ w_in[l]
        q, k, v, ca, cg = jnp.split(proj, [ATTN_WIDTH, 2 * ATTN_WIDTH, 3 * ATTN_WIDTH,
                                           3 * ATTN_WIDTH + CONV_CHANNELS], axis=-1)
        q = q.reshape(B, S, N_ATTN_HEADS, 2, QK_HEAD_DIM)
        k = k.reshape(B, S, N_ATTN_HEADS, 2, QK_HEAD_DIM)
        v = v.reshape(B, S, N_ATTN_HEADS, V_HEAD_DIM)
        attn_out = diff_attention(q, k, v, lam, lam0, subln_g[l])
        conv_out = conformer_conv(ca, cg, conv_w[l], conv_b[l], conv_ln_g[l], conv_ln_b[l])
        h = h + jnp.concatenate([attn_out, conv_out], axis=-1) @ w_out[l]
        h = h + hier_moe(rmsnorm(h, norm_ffn_g[l]), w_group_router[l], b_group_router[l],
                         w_expert_router[l], b_expert_router[l], w_gate[l], w_up[l], w_down[l])
    return rmsnorm(h, norm_final_g)
```

```python
import math
from contextlib import ExitStack
import numpy as np
import ml_dtypes
import concourse.bass as bass
import concourse.mybir as mybir
from concourse.bass_utils import run_bass_kernel_spmd

F32 = mybir.dt.float32
BF16 = mybir.dt.bfloat16
I32 = mybir.dt.int32
AF = mybir.ActivationFunctionType
ALU = mybir.AluOpType
AX = mybir.AxisListType

D = 2048
KC = 16
H = 8
NE = 32
EPS = 1e-6
TAPS = 31
ENG = ['pe', 'act', 'dve', 'pool', 'sp']
SAME_ENGINE_SYNC = True


class Buf:
    __slots__ = ('name', 'wc', 'wd', 'rc', 'rd')

    def __init__(self, name):
        self.name = name
        self.wc = {}
        self.wd = {}
        self.rc = {}
        self.rd = {}


class Op:
    __slots__ = ('eng', 'fn', 'cdeps', 'ddeps', 'sig', 'sigval', 'dma', 'emitted')


class Prog:
    def __init__(self, nc, stack):
        self.nc = nc
        self.stack = stack
        self.ops = {e: [] for e in ENG}
        self.csem = {e: stack.enter_context(nc.semaphore("cs_" + e)) for e in ENG}
        self.ccount = {e: 0 for e in ENG}
        self.waited = {e: {} for e in ENG}
        self.dsems = {}
        self.nsem = len(ENG)

    def dsem(self, key):
        ent = self.dsems.get(key)
        if ent is None:
            ent = [self.stack.enter_context(self.nc.semaphore("ds%d" % len(self.dsems))), 0]
            self.dsems[key] = ent
            self.nsem += 1
        return ent

    def add(self, eng, fn, r=(), w=(), dkey=None, ww=()):
        op = Op()
        op.eng, op.fn, op.cdeps, op.ddeps = eng, fn, [], []
        op.sig, op.sigval, op.dma, op.emitted = False, None, None, False
        if dkey is not None:
            ent = self.dsem(dkey)
            ent[1] += 16
            op.dma = (ent[0], ent[1])

        def cdep(d):
            if d is op:
                return
            if d.eng != eng or op.dma is not None or (SAME_ENGINE_SYNC and eng != 'pe'):
                op.cdeps.append(d)
                if not d.emitted:
                    d.sig = True

        for b in r:
            for d in b.wc.values():
                cdep(d)
            for sv in b.wd.items():
                op.ddeps.append(sv)
        for b in w:
            for d in b.wc.values():
                cdep(d)
            for d in b.rc.values():
                cdep(d)
            for sv in b.wd.items():
                op.ddeps.append(sv)
            for sv in b.rd.items():
                op.ddeps.append(sv)
        for b in r:
            if b in w:
                continue
            if op.dma is not None:
                b.rd[op.dma[0]] = op.dma[1]
            else:
                b.rc[eng] = op
        for b in w:
            b.rc.clear()
            b.rd.clear()
            b.wc.clear()
            b.wd.clear()
            if op.dma is not None:
                b.wd[op.dma[0]] = op.dma[1]
            else:
                b.wc[eng] = op
        for b in ww:
            if op.dma is not None:
                b.wd[op.dma[0]] = op.dma[1]
            else:
                b.wc[eng] = op
        self.ops[eng].append(op)
        return op

    def emit(self, name):
        nc = self.nc
        for e in ENG:
            for op in self.ops[e]:
                if op.sig and op.dma is None:
                    self.ccount[e] += 1
                    op.sigval = self.ccount[e]
        endval = {}
        for e in ENG:
            self.ccount[e] += 1
            endval[e] = self.ccount[e]
        ops, csem, waited = self.ops, self.csem, self.waited

        def run(e, eng):
            wt = waited[e]
            for op in ops[e]:
                for d in op.cdeps:
                    if d.sigval is None:
                        continue
                    s = csem[d.eng]
                    if wt.get(s, 0) < d.sigval:
                        eng.wait_ge(s, d.sigval)
                        wt[s] = d.sigval
                for (s, v) in op.ddeps:
                    if wt.get(s, 0) < v:
                        eng.wait_ge(s, v)
                        wt[s] = v
                ins = op.fn(eng)
                if op.dma is not None:
                    ins.then_inc(op.dma[0], 16)
                elif op.sig:
                    ins.then_inc(csem[e], 1)
                op.emitted = True
            eng.drain().then_inc(csem[e], 1)
            for e2 in ENG:
                if e2 != e:
                    eng.wait_ge(csem[e2], endval[e2])
                    wt[csem[e2]] = endval[e2]

        with nc.Block(name) as block:
            @block.tensor
            def _(eng):
                run('pe', eng)

            @block.scalar
            def _(eng):
                run('act', eng)

            @block.vector
            def _(eng):
                run('dve', eng)

            @block.gpsimd
            def _(eng):
                run('pool', eng)

            @block.sync
            def _(eng):
                run('sp', eng)
        self.ops = {e: [] for e in ENG}

    def mm(self, out, lhsT, rhs, start, stop, r=(), w=(), ww=()):
        return self.add('pe', lambda e: e.matmul(out, lhsT, rhs, start=start, stop=stop), r, w, ww=ww)

    def tr(self, out, in_, ident, r=(), w=()):
        return self.add('pe', lambda e: e.transpose(out, in_, ident), r, w)

    def act(self, out, in_, func, r=(), w=(), ww=(), **kw):
        return self.add('act', lambda e: e.activation(out, in_, func, **kw), r, w, ww=ww)

    def ts(self, eng, out, in0, s1, s2, op0, op1=None, r=(), w=(), ww=(), **kw):
        if op1 is None:
            return self.add(eng, lambda e: e.tensor_scalar(out, in0, s1, None, op0, **kw), r, w, ww=ww)
        return self.add(eng, lambda e: e.tensor_scalar(out, in0, s1, s2, op0, op1, **kw), r, w, ww=ww)

    def tt(self, eng, out, in0, in1, op, r=(), w=(), ww=()):
        return self.add(eng, lambda e: e.tensor_tensor(out, in0, in1, op), r, w, ww=ww)

    def stt(self, out, in0, scalar, in1, op0, op1, r=(), w=(), **kw):
        return self.add('dve', lambda e: e.scalar_tensor_tensor(out, in0, scalar, in1, op0, op1, **kw), r, w)

    def ttr(self, out, in0, in1, accum, r=(), w=()):
        return self.add('dve', lambda e: e.scalar_tensor_tensor(out, in0, 1.0, in1, ALU.mult, ALU.mult, accum_out=accum), r, w)

    def cp(self, eng, out, in_, r=(), w=(), ww=()):
        if eng == 'act':
            return self.add('act', lambda e: e.copy(out, in_), r, w, ww=ww)
        return self.add(eng, lambda e: e.tensor_copy(out, in_), r, w, ww=ww)

    def dma(self, q, out, in_, r=(), w=(), key=None, ww=(), **kw):
        return self.add(q, lambda e: e.dma_start(out, in_, **kw), r, w, dkey=key, ww=ww)


def _slopes():
    return [2.0 ** (-8.0 * (i + 1) / H) for i in range(H)]


def build(S=4096, CAP=384, dbg=False, nph=99):
    NT = S // 128
    NQ = S // 512
    NHALF = max(1, S // 2048)
    HS = S // NHALF
    ND = NT + 3
    NST = CAP // 128
    nc = bass.Bass("TRN2", target_bir_lowering=False)

    def din(name, shape, dt=F32):
        return nc.dram_tensor(name, list(shape), dt, kind="ExternalInput")

    def dscr(name, shape, dt):
        return nc.dram_tensor(name, list(shape), dt, kind="ExternalOutput" if dbg else "Internal")

    x = din("x", [S, D])
    w_in = din("w_in", [D, 5120])
    w_out = din("w_out", [D, D])
    w_gate = din("w_gate", [NE, D, 1024])
    w_up = din("w_up", [NE, D, 1024])
    w_down = din("w_down", [NE, 1024, D])
    cf_d = din("cf", [128, 416 + 8 * ND])
    cb_d = din("cb", [128, 512], BF16)
    qaug_d = din("qaug", [H, 2, S], BF16)
    pp_d = din("pp", [128, 16 + 8 * TAPS + 24 + 1 + 256])
    bc_d = din("bc", [128, 2 * D + 36])
    wr_d = din("wr", [128, KC * 36])
    out_d = nc.dram_tensor("out", [S, D], F32, kind="ExternalOutput")

    QT = dscr("QT", [H, 128, S], BF16)
    KT = dscr("KT", [H, 128, S], BF16)
    VV = dscr("VV", [S, 1024], BF16)
    UT = dscr("UT", [1024, S], BF16)
    MIXT = dscr("MIXT", [D, S], BF16)
    HH = dscr("HH", [S, D], F32)
    XS = dscr("XS", [NE * CAP, D], BF16)
    YS = dscr("YS", [NE * CAP, D], F32)
    RT = dscr("RT", [128, 4 * NT], F32)

    top = ExitStack()
    with top:
        P = Prog(nc, top)

        def sb(stack, name, shape, dt):
            return stack.enter_context(nc.sbuf_tensor("s_" + name, list(shape), dt))

        def ps(stack, name, shape, dt=F32):
            return stack.enter_context(nc.psum_tensor("p_" + name, list(shape), dt))

        cf = sb(top, "cf", [128, 416 + 8 * ND], F32)
        cb = sb(top, "cb", [128, 512], BF16)
        pp = sb(top, "pp", [128, 16 + 8 * TAPS + 24 + 1 + 256], F32)
        wr = sb(top, "wr", [128, KC * 36], F32)
        rt = sb(top, "rt", [128, 4 * NT], F32)
        rti = sb(top, "rti", [128, 2 * NT], I32)
        sm = sb(top, "sm", [128, 16], F32)
        b_const = Buf("const")
        b_rt = Buf("rt")
        ident = cf[:, 0:128]
        triU = cf[:, 128:256]
        ones_f = cf[:, 256:384]
        ebase = cf[:, 384:416]
        ALIB = 416
        ident_b = cb[:, 0:128]
        ones_b = cb[:, 128:256]
        tri_b = cb[:, 256:384]
        negtri_b = cb[:, 384:512]
        PP_G = 0
        PP_CW = 16
        PP_CB = PP_CW + 8 * TAPS
        PP_LG = PP_CB + 8
        PP_LB = PP_LG + 8
        PP_SG = PP_LB + 8
        PP_LAM = PP_SG + 1
        neglam = sm[:, 0:1]
        g08 = sm[:, 1:2]
        mhalf = sm[:, 2:3]

        b_sm = Buf("sm")
        P.dma('sp', cf[:, :], cf_d[:, :], w=[b_const], key="c0")
        P.dma('sp', cb[:, :], cb_d[:, :], w=[b_const], key="c0")
        P.dma('sp', pp[:, :], pp_d[:, :], w=[b_const], key="c0")
        P.dma('sp', wr[:, :], wr_d[:, :], w=[b_const], key="c0")
        lq = PP_LAM
        P.add('dve', lambda e: e.memset(sm[:, :], 0.0), w=[b_sm])
        P.add('dve', lambda e: e.memset(mhalf, -0.5), w=[b_sm])
        with ExitStack() as st0:
            junk = sb(st0, "junk0", [128, 64], F32)
            b_j = Buf("junk0")
            P.ttr(junk[:, :], pp[:, lq:lq + 64], pp[:, lq + 64:lq + 128], sm[:, 4:5], r=[b_const], w=[b_sm, b_j])
            P.ttr(junk[:, :], pp[:, lq + 128:lq + 192], pp[:, lq + 192:lq + 256], sm[:, 5:6], r=[b_const], w=[b_sm, b_j])
            P.act(sm[:, 6:8], sm[:, 4:6], AF.Exp, r=[b_sm], w=[b_sm])
            P.tt('dve', sm[:, 3:4], sm[:, 7:8], sm[:, 6:7], ALU.subtract, r=[b_sm], w=[b_sm])
            P.ts('dve', neglam, sm[:, 3:4], -(0.8 - 0.6), None, ALU.add, r=[b_sm], w=[b_sm])
            P.ts('dve', g08, pp[:, PP_SG:PP_SG + 1], 1.0 - (0.8 - 0.6), None, ALU.mult, r=[b_sm, b_const], w=[b_sm])
            P.emit("ph0")

        def phase1():
            with ExitStack() as st:
                CS = min(1024, S)
                NCH = S // CS
                ntl = CS // 128
                xnT2 = [sb(st, "xnT%d" % i, [128, KC, CS], BF16) for i in range(2)]
                NW = 4
                Wb = [sb(st, "Wb%d" % i, [128, KC, 512], BF16) for i in range(NW)]
                stg = [sb(st, "stg%d" % i, [128, CS], BF16) for i in range(3)]
                xt = [sb(st, "xt%d" % i, [128, D], F32) for i in range(2)]
                jk = sb(st, "jk", [128, D], BF16)
                sgt = [sb(st, "sgt%d" % i, [128, 512], F32) for i in range(2)]
                st1 = sb(st, "st1", [128, 8], F32)
                tp = [ps(st, "tp%d" % i, [128, 512]) for i in range(2)]
                pj = [ps(st, "pj%d" % i, [128, 512]) for i in range(4)]
                b_xnT2 = [[Buf("xnT%d_%d" % (u, i)) for i in range(ntl)] for u in range(2)]
                b_W = [Buf("W%d" % i) for i in range(NW)]
                b_stg = [Buf("stg%d" % i) for i in range(3)]
                b_xt = [Buf("xt%d" % i) for i in range(2)]
                b_jk = Buf("jk")
                b_sgt = [Buf("sgt%d" % i) for i in range(2)]
                b_st1 = [Buf("st1a"), Buf("st1b")]
                b_tp = [Buf("tp0"), Buf("tp1")]
                b_pj = [Buf("pj%d" % i) for i in range(4)]
                b_scr = Buf("scr1")
                cnt = {'w': 0, 'stg': 0, 'pj': 0, 'ev': 0}

                def evac(out, in_, r, w):
                    cnt['ev'] += 1
                    if cnt['ev'] % 2:
                        P.cp('act', out, in_, r=r, w=w)
                    else:
                        P.cp('dve', out, in_, r=r, w=w)

                def wload(c0):
                    s = cnt['w'] % NW
                    cnt['w'] += 1
                    src = w_in[:, c0:c0 + 512].rearrange("(k p) c -> p k c", p=128)
                    for kq in range(4):
                        P.dma('pool', Wb[s][:, kq * 4:(kq + 1) * 4, :], src[:, kq * 4:(kq + 1) * 4, :], w=[b_W[s]], key=("W", s))
                    return s

                def xload(gt):
                    P.dma('sp', xt[gt % 2][:, :], x[gt * 128:(gt + 1) * 128, :], w=[b_xt[gt % 2]], key=("xt", gt % 2))

                def norm_pre(gt):
                    xs_, bx = xt[gt % 2], b_xt[gt % 2]
                    a = (gt % 2) * 4
                    bs = b_st1[gt % 2]
                    P.ttr(jk[:, :], xs_[:, :], xs_[:, :], st1[:, a:a + 1], r=[bx], w=[b_jk, bs])
                    P.ts('dve', st1[:, a + 1:a + 2], st1[:, a:a + 1], 1.0 / D, EPS, ALU.mult, ALU.add, r=[bs], w=[bs])
                    P.tt('pool', st1[:, a + 2:a + 3], st1[:, a + 1:a + 2], mhalf, ALU.pow, r=[bs, b_sm], w=[bs])
                    P.ts('dve', xs_[:, :], xs_[:, :], st1[:, a + 2:a + 3], None, ALU.mult, r=[bs, bx], w=[bx])

                def norm_tr(gt):
                    c, i = gt // ntl, gt % ntl
                    xnT, b_xnT = xnT2[c % 2], b_xnT2[c % 2]
                    xs_, bx = xt[gt % 2], b_xt[gt % 2]
                    for g4 in range(4):
                        tpi, btp = tp[g4 % 2], b_tp[g4 % 2]
                        for j in range(4):
                            k = g4 * 4 + j
                            P.tr(tpi[:, j * 128:(j + 1) * 128], xs_[:, k * 128:(k + 1) * 128], ident, r=[bx, b_const], w=[btp])
                        for j in range(4):
                            k = g4 * 4 + j
                            o = xnT[:, k, i * 128:(i + 1) * 128]
                            gcol = pp[:, PP_G + k:PP_G + k + 1]
                            if j % 2 == 0:
                                P.act(o, tpi[:, j * 128:(j + 1) * 128], AF.Copy, r=[btp, b_const], w=[b_xnT[i]], scale=gcol)
                            else:
                                P.ts('dve', o, tpi[:, j * 128:(j + 1) * 128], gcol, None, ALU.mult, r=[btp, b_const], w=[b_xnT[i]])

                def step(k):
                    norm_tr(k)
                    if k + 2 < NT:
                        xload(k + 2)
                    if k + 1 < NT:
                        norm_pre(k + 1)

                worder = []
                for c_ in range(NCH):
                    worder += [0, 512, 1024, 1536, 2048, 2560, 3072, 4096, 3584, 4608]
                wstate = {'issued': 0}

                def need(idx):
                    while wstate['issued'] < min(len(worder), idx + 3):
                        wload(worder[wstate['issued']])
                        wstate['issued'] += 1
                    return idx % NW

                xload(0)
                if NT > 1:
                    xload(1)
                norm_pre(0)
                for gt in range(ntl):
                    step(gt)
                for c in range(NCH):
                    t0 = c * CS
                    xnT, b_xnT = xnT2[c % 2], b_xnT2[c % 2]
                    nxt_tiles = list(range((c + 1) * ntl, (c + 2) * ntl)) if c + 1 < NCH else []

                    def interleave():
                        if nxt_tiles:
                            step(nxt_tiles.pop(0))

                    def feat_major(ws, sub, dst_ap):
                        si = cnt['stg'] % 3
                        cnt['stg'] += 1
                        for tq in range(CS // 512):
                            pi = cnt['pj'] % 4
                            cnt['pj'] += 1
                            for k in range(KC):
                                P.mm(pj[pi][:, :], Wb[ws][:, k, sub * 128:(sub + 1) * 128], xnT[:, k, tq * 512:(tq + 1) * 512],
                                     k == 0, k == KC - 1, r=[b_W[ws]] + b_xnT[tq * 4:tq * 4 + 4], w=[b_pj[pi]])
                            evac(stg[si][:, tq * 512:(tq + 1) * 512], pj[pi][:, :], r=[b_pj[pi]], w=[b_stg[si]])
                        P.dma('sp', dst_ap, stg[si][:, :], r=[b_stg[si]], w=[b_scr], key=("stg", si))

                    wb0 = c * 10
                    for g in range(4):
                        ws = need(wb0 + g)
                        for sub in range(4):
                            hh = (g % 2) * 4 + sub
                            dst = (QT if g < 2 else KT)[hh, :, t0:t0 + CS]
                            feat_major(ws, sub, dst)
                        interleave()
                    for g in range(2):
                        ws = need(wb0 + 4 + g)
                        for i in range(ntl):
                            pi = cnt['pj'] % 4
                            cnt['pj'] += 1
                            for k in range(KC):
                                P.mm(pj[pi][:, :], xnT[:, k, i * 128:(i + 1) * 128], Wb[ws][:, k, :], k == 0, k == KC - 1,
                                     r=[b_W[ws], b_xnT[i]], w=[b_pj[pi]])
                            si = cnt['stg'] % 3
                            cnt['stg'] += 1
                            evac(stg[si][:, 0:512], pj[pi][:, :], r=[b_pj[pi]], w=[b_stg[si]])
                            P.dma('sp', VV[t0 + i * 128:t0 + (i + 1) * 128, g * 512:(g + 1) * 512], stg[si][:, 0:512],
                                  r=[b_stg[si]], w=[b_scr], key=("stg", si))
                        interleave()
                    for g in range(2):
                        wa = need(wb0 + 6 + 2 * g)
                        wg = need(wb0 + 7 + 2 * g)
                        for sub in range(4):
                            si = cnt['stg'] % 3
                            cnt['stg'] += 1
                            for tq in range(CS // 512):
                                pa = cnt['pj'] % 4
                                pg = (cnt['pj'] + 1) % 4
                                cnt['pj'] += 2
                                rr_ = b_xnT[tq * 4:tq * 4 + 4]
                                for k in range(KC):
                                    P.mm(pj[pa][:, :], Wb[wa][:, k, sub * 128:(sub + 1) * 128], xnT[:, k, tq * 512:(tq + 1) * 512],
                                         k == 0, k == KC - 1, r=[b_W[wa]] + rr_, w=[b_pj[pa]])
                                for k in range(KC):
                                    P.mm(pj[pg][:, :], Wb[wg][:, k, sub * 128:(sub + 1) * 128], xnT[:, k, tq * 512:(tq + 1) * 512],
                                         k == 0, k == KC - 1, r=[b_W[wg]] + rr_, w=[b_pj[pg]])
                                sgi = tq % 2
                                P.act(sgt[sgi][:, :], pj[pg][:, :], AF.Sigmoid, r=[b_pj[pg]], w=[b_sgt[sgi]])
                                P.tt('dve', stg[si][:, tq * 512:(tq + 1) * 512], pj[pa][:, :], sgt[sgi][:, :], ALU.mult,
                                     r=[b_pj[pa], b_sgt[sgi]], w=[b_stg[si]])
                            ch0 = g * 512 + sub * 128
                            P.dma('sp', UT[ch0:ch0 + 128, t0:t0 + CS], stg[si][:, :], r=[b_stg[si]], w=[b_scr], key=("stg", si))
                            if sub % 2 == 1:
                                interleave()
                    while nxt_tiles:
                        interleave()
                P.emit("ph1")
            return b_scr

        b_scr1 = phase1()

        def phase2():
            with ExitStack() as st:
                Dg = sb(st, "Dg", [128, 8 * TAPS, 128], BF16)
                U = [sb(st, "U%d" % i, [128, 8, 544], BF16) for i in range(2)]
                csb2 = [sb(st, "csb%d" % i, [128, 8, 512], F32) for i in range(2)]
                csq2 = [sb(st, "csq%d" % i, [128, 8, 512], F32) for i in range(2)]
                mean_sb = sb(st, "mean_sb", [128, 512], F32)
                msq = sb(st, "msq", [128, 512], F32)
                var = sb(st, "var", [128, 512], F32)
                rstd = sb(st, "rstd", [128, 512], F32)
                tmp = [sb(st, "tmp%d" % i, [128, 512], F32) for i in range(2)]
                tm2 = [sb(st, "tm2%d" % i, [128, 512], F32) for i in range(2)]
                ostg = [sb(st, "ostg%d" % i, [128, 512], BF16) for i in range(2)]
                pc = [ps(st, "pc%d" % i, [128, 512]) for i in range(2)]
                pst = [ps(st, "pst%d" % i, [128, 512]) for i in range(2)]
                b_Dg = [Buf("Dg%d" % i) for i in range(8)]
                b_U = [Buf("U0"), Buf("U1")]
                b_csb2 = [[Buf("csb%d_%d" % (u, i)) for i in range(8)] for u in range(2)]
                b_csq2 = [[Buf("csq%d_%d" % (u, i)) for i in range(8)] for u in range(2)]
                b_pc = [Buf("pc0"), Buf("pc1")]
                b_pst = [Buf("pst0"), Buf("pst1")]
                b_stat = Buf("stat")
                b_tmp = [Buf("tmp0"), Buf("tmp1")]
                b_tm2 = [Buf("tm20"), Buf("tm21")]
                b_ostg = [Buf("ostg0"), Buf("ostg1")]
                b_out = Buf("scr2")
                for cc in range(8):
                    for j in range(TAPS):
                        col = PP_CW + cc * TAPS + j
                        eng = 'dve'
                        P.ts(eng, Dg[:, cc * TAPS + j, :], ident, pp[:, col:col + 1], None, ALU.mult,
                             r=[b_const], ww=[b_Dg[cc]])
                UTv = UT.ap().rearrange("(c p) t -> p c t", p=128)

                def uload(tt):
                    u = tt % 2
                    if tt == 0:
                        P.add('pool', lambda e: e.memset(U[0][:, :, 0:30], 0.0), w=[b_U[0]])
                        P.dma('sp', U[0][:, :, 30:542], UTv[:, :, 0:512], r=[b_scr1], w=[b_U[0]], key=("U", 0))
                    else:
                        P.dma('sp', U[u][:, :, 0:542], UTv[:, :, tt * 512 - 30:tt * 512 + 512], r=[b_scr1], w=[b_U[u]], key=("U", u))
                uload(0)
                nn = {'n': 0}

                def ln_apply(tt, cc):
                    csb, b_csb = csb2[tt % 2], b_csb2[tt % 2]
                    ti = nn['n'] % 2
                    nn['n'] += 1
                    P.tt('dve', tmp[ti][:, :], csb[:, cc, :], mean_sb[:, :], ALU.subtract, r=[b_csb[cc], b_stat], w=[b_tmp[ti]])
                    P.tt('dve', tm2[ti][:, :], tmp[ti][:, :], rstd[:, :], ALU.mult, r=[b_tmp[ti], b_stat], w=[b_tm2[ti]])
                    P.act(ostg[ti][:, :], tm2[ti][:, :], AF.Silu, r=[b_tm2[ti], b_const], w=[b_ostg[ti]],
                          scale=pp[:, PP_LG + cc:PP_LG + cc + 1], bias=pp[:, PP_LB + cc:PP_LB + cc + 1])
                    P.dma('sp', MIXT[1024 + cc * 128:1024 + (cc + 1) * 128, tt * 512:(tt + 1) * 512], ostg[ti][:, :],
                          r=[b_ostg[ti]], ww=[b_out], key=("ostg", ti))

                for tt in range(NQ):
                    if tt + 1 < NQ:
                        uload(tt + 1)
                    u = tt % 2
                    csb, csq, b_csb, b_csq = csb2[u], csq2[u], b_csb2[u], b_csq2[u]
                    for cc in range(8):
                        pi = cc % 2
                        for j in range(TAPS):
                            P.mm(pc[pi][:, :], Dg[:, cc * TAPS + j, :], U[u][:, cc, j:j + 512], j == 0, j == TAPS - 1,
                                 r=[b_Dg[cc], b_U[u]], w=[b_pc[pi]])
                        cbias = pp[:, PP_CB + cc:PP_CB + cc + 1]
                        P.act(csb[:, cc, :], pc[pi][:, :], AF.Identity, r=[b_pc[pi], b_const], w=[b_csb[cc]], bias=cbias)
                        P.act(csq[:, cc, :], pc[pi][:, :], AF.Square, r=[b_pc[pi], b_const], w=[b_csq[cc]], bias=cbias)
                        P.mm(pst[0][:, :], ones_f, csb[:, cc, :], cc == 0, cc == 7, r=[b_csb[cc], b_const], w=[b_pst[0]])
                        P.mm(pst[1][:, :], ones_f, csq[:, cc, :], cc == 0, cc == 7, r=[b_csq[cc], b_const], w=[b_pst[1]])
                        if tt > 0:
                            ln_apply(tt - 1, cc)
                    P.ts('dve', mean_sb[:, :], pst[0][:, :], 1.0 / 1024, None, ALU.mult, r=[b_pst[0]], w=[b_stat])
                    P.tt('dve', msq[:, :], mean_sb[:, :], mean_sb[:, :], ALU.mult, r=[b_stat], w=[b_stat])
                    P.stt(var[:, :], pst[1][:, :], 1.0 / 1024, msq[:, :], ALU.mult, ALU.subtract, r=[b_pst[1], b_stat], w=[b_stat])
                    P.ts('dve', var[:, :], var[:, :], EPS, None, ALU.add, r=[b_stat], w=[b_stat])
                    P.act(msq[:, :], var[:, :], AF.Ln, r=[b_stat], w=[b_stat])
                    P.act(rstd[:, :], msq[:, :], AF.Exp, r=[b_stat], w=[b_stat], scale=-0.5)
                for cc in range(8):
                    ln_apply(NQ - 1, cc)
                P.emit("ph2")
            return b_out

        b_scr2 = phase2() if nph >= 2 else None

        def phase3():
            with ExitStack() as st:
                QA = [[sb(st, "QA%d_%d" % (c, i), [128, S], BF16) for i in range(2)] for c in range(2)]
                KA = [[sb(st, "KA%d_%d" % (c, i), [128, S], BF16) for i in range(2)] for c in range(2)]
                Vt = [sb(st, "Vt%d" % i, [128, NT, 128], BF16) for i in range(2)]
                NPB, NSB = 4, 3
                PT = [sb(st, "PT%d" % i, [128, 512], BF16) for i in range(NPB)]
                rr = [sb(st, "rr%d" % i, [128, 512], F32) for i in range(2)]
                tq_ = [sb(st, "tq%d" % i, [128, 512], F32) for i in range(2)]
                A = [sb(st, "A%d" % i, [128, 256], F32) for i in range(2)]
                Asq = [sb(st, "Asq%d" % i, [128, 256], F32) for i in range(2)]
                lnv = [sb(st, "lnv%d" % i, [128, 256], F32) for i in range(2)]
                ostg = [sb(st, "aostg%d" % i, [128, 256], BF16) for i in range(2)]
                sT = [ps(st, "sT%d" % i, [128, 512]) for i in range(NSB)]
                OO = [ps(st, "OO%d" % i, [128, 512]) for i in range(2)]
                LL = [ps(st, "LL%d" % i, [128, 512]) for i in range(2)]
                ssb = ps(st, "ssb", [128, 512])
                b_QA = [Buf("QA0"), Buf("QA1")]
                b_KA = [Buf("KA0"), Buf("KA1")]
                b_Vt = [Buf("Vt0"), Buf("Vt1")]
                b_PT = [Buf("PT%d" % i) for i in range(NPB)]
                b_sT = [Buf("sT%d" % i) for i in range(NSB)]
                b_OO = [Buf("OO0"), Buf("OO1")]
                b_LL = [Buf("LL0"), Buf("LL1")]
                b_ssb = Buf("ssb")
                b_rr = [Buf("rr0"), Buf("rr1")]
                b_tq = [Buf("tq0"), Buf("tq1")]
                b_A = [Buf("A0"), Buf("A1")]
                b_Asq = [Buf("Asq0"), Buf("Asq1")]
                b_lnv = [Buf("lnv0"), Buf("lnv1")]
                b_ostg = [Buf("aostg0"), Buf("aostg1")]
                b_out = Buf("scr3")
                for i in range(2):
                    P.add('pool', lambda e, i=i: e.memset(QA[1][i][0:64, :], 0.0), w=[b_QA[i]])
                    P.add('pool', lambda e, i=i: e.memset(QA[0][i][64:128, :], 0.0), w=[b_QA[i]])
                    P.add('pool', lambda e, i=i: e.memset(KA[1][i][0:64, :], 0.0), w=[b_KA[i]])
                    P.add('pool', lambda e, i=i: e.memset(KA[0][i][64:128, :], 0.0), w=[b_KA[i]])
                    P.add('pool', lambda e, i=i: e.memset(KA[0][i][64:66, :], 1.0), w=[b_KA[i]])
                    P.add('pool', lambda e, i=i: e.memset(KA[1][i][32:34, :], 1.0), w=[b_KA[i]])
                VVv = VV.ap().rearrange("(n p) d -> p n d", p=128)
                mh256 = sb(st, "mh256", [128, 256], F32)
                b_mh = Buf("mh256")
                P.add('pool', lambda e: e.memset(mh256[:, :], -0.5), w=[b_mh])

                def hload(h):
                    s_ = h % 2
                    P.dma('sp', QA[0][s_][0:64, :], QT[h, 0:64, :], r=[b_scr1], w=[b_QA[s_]], key=("QA", s_))
                    P.dma('sp', QA[1][s_][64:128, :], QT[h, 64:128, :], r=[b_scr1], ww=[b_QA[s_]], key=("QA", s_))
                    P.dma('sp', QA[0][s_][64:66, :], qaug_d[h, :, :], ww=[b_QA[s_]], key=("QA", s_))
                    P.dma('sp', QA[1][s_][32:34, :], qaug_d[h, :, :], ww=[b_QA[s_]], key=("QA", s_))
                    P.dma('sp', KA[0][s_][0:64, :], KT[h, 0:64, :], r=[b_scr1], w=[b_KA[s_]], key=("KA", s_))
                    P.dma('sp', KA[1][s_][64:128, :], KT[h, 64:128, :], r=[b_scr1], ww=[b_KA[s_]], key=("KA", s_))
                    P.dma('sp', Vt[s_][:, :, :], VVv[:, :, h * 128:(h + 1) * 128], r=[b_scr1], w=[b_Vt[s_]], key=("Vt", s_))

                NQ2 = S // 256
                blocks = []
                nqt = 0
                for h in range(H):
                    for qt in range(NQ2):
                        nkb = 2 * (qt + 1)
                        for kb in range(nkb):
                            j = kb - 2 * qt
                            blocks.append(dict(h=h, qt=qt, kb=kb, j=j, cs=(j * 128 if j >= 0 else 0), nkb=nkb,
                                               sb=len(blocks) % NSB, pb=len(blocks) % NPB, ob=nqt % 2, ep=nqt))
                        nqt += 1

                def qk(b):
                    s_, cs, q0 = b['h'] % 2, b['cs'], b['qt'] * 256
                    diag = b['j'] >= 0
                    for c in range(2):
                        P.mm(sT[b['sb']][:, c * 256 + cs:(c + 1) * 256], KA[c][s_][:, b['kb'] * 128:(b['kb'] + 1) * 128],
                             QA[c][s_][:, q0 + cs:q0 + 256], True, not diag, r=[b_KA[s_], b_QA[s_]],
                             w=[b_sT[b['sb']]] if c == 0 else [], ww=[] if c == 0 else [b_sT[b['sb']]])
                        if diag:
                            d0 = c * 256 + cs
                            P.mm(sT[b['sb']][:, d0:d0 + 128], ident_b, negtri_b, False, True, r=[b_const], ww=[b_sT[b['sb']]])

                def rest(b):
                    h, s_, cs, kb, nkb = b['h'], b['h'] % 2, b['cs'], b['kb'], b['nkb']
                    sbi, pb, ob = b['sb'], b['pb'], b['ob']
                    col = ALIB + h * ND + (2 * b['qt'] - kb + 3)
                    bias = cf[:, col:col + 1]
                    if cs == 0:
                        rngs = [(0, 512)]
                    else:
                        rngs = [(cs, 256), (256 + cs, 512)]
                    for ri, (a0, a1) in enumerate(rngs):
                        P.act(PT[pb][:, a0:a1], sT[sbi][:, a0:a1], AF.Exp, r=[b_sT[sbi], b_const],
                              w=[b_PT[pb]] if ri == 0 else [], ww=[] if ri == 0 else [b_PT[pb]], bias=bias, scale=0.125)
                    for ri, (a0, a1) in enumerate(rngs):
                        P.mm(OO[ob][:, a0:a1], Vt[s_][:, kb, :], PT[pb][:, a0:a1], kb == 0, kb == nkb - 1,
                             r=[b_Vt[s_], b_PT[pb]], w=[b_OO[ob]])
                        P.mm(LL[ob][:, a0:a1], ones_b, PT[pb][:, a0:a1], kb == 0, kb == nkb - 1,
                             r=[b_PT[pb], b_const], w=[b_LL[ob]])

                def epi1(b):
                    ob, e2 = b['ob'], b['ep'] % 2
                    P.add('dve', lambda e: e.reciprocal(rr[e2][:, :], LL[ob][:, :]), r=[b_LL[ob]], w=[b_rr[e2]])
                    P.tt('dve', tq_[e2][:, :], OO[ob][:, :], rr[e2][:, :], ALU.mult, r=[b_OO[ob], b_rr[e2]], w=[b_tq[e2]])
                    P.stt(A[e2][:, :], tq_[e2][:, 256:512], neglam, tq_[e2][:, 0:256], ALU.mult, ALU.add, r=[b_tq[e2], b_sm], w=[b_A[e2]])
                    P.tt('dve', Asq[e2][:, :], A[e2][:, :], A[e2][:, :], ALU.mult, r=[b_A[e2]], w=[b_Asq[e2]])

                def epi2(b):
                    e2, h, q0 = b['ep'] % 2, b['h'], b['qt'] * 256
                    P.mm(ssb[:, 0:256], ones_f, Asq[e2][:, :], True, True, r=[b_Asq[e2], b_const], w=[b_ssb])
                    P.ts('dve', lnv[e2][:, :], ssb[:, 0:256], 1.0 / 128, EPS, ALU.mult, ALU.add, r=[b_ssb], w=[b_lnv[e2]])
                    P.act(Asq[e2][:, :], lnv[e2][:, :], AF.Ln, r=[b_lnv[e2]], w=[b_Asq[e2]])
                    P.act(lnv[e2][:, :], Asq[e2][:, :], AF.Exp, r=[b_Asq[e2]], w=[b_lnv[e2]], scale=-0.5)
                    P.stt(ostg[e2][:, :], A[e2][:, :], g08, lnv[e2][:, :], ALU.mult, ALU.mult, r=[b_A[e2], b_lnv[e2], b_sm], w=[b_ostg[e2]])
                    P.dma('sp', MIXT[h * 128:(h + 1) * 128, q0:q0 + 256], ostg[e2][:, :], r=[b_ostg[e2]], ww=[b_out], key=("aostg", e2))

                hload(0)
                if H > 1:
                    hload(1)
                wov = w_out.ap().rearrange("(k p) c -> p k c", p=128)
                for kq in range(4):
                    for ch in range(2):
                        P.dma('pool', wo[:, kq * 4:(kq + 1) * 4, ch * 1024:(ch + 1) * 1024],
                              wov[:, kq * 4:(kq + 1) * 4, ch * 1024:(ch + 1) * 1024], ww=[b_wo], key="wo")
                LOOK = NSB - 1
                pend = []
                nb = len(blocks)
                for i in range(min(LOOK, nb)):
                    qk(blocks[i])
                for i in range(nb):
                    if i + LOOK < nb:
                        qk(blocks[i + LOOK])
                    b = blocks[i]
                    rest(b)
                    pend = [(cnt - 1, bb) for (cnt, bb) in pend]
                    for (cnt, bb) in pend:
                        if cnt <= 0:
                            epi2(bb)
                    pend = [(cnt, bb) for (cnt, bb) in pend if cnt > 0]
                    if b['kb'] == b['nkb'] - 1:
                        while len(pend) >= 2:
                            epi2(pend.pop(0)[1])
                        epi1(b)
                        pend.append((12, b))
                        if b['qt'] == NQ2 - 1 and b['h'] + 2 < H:
                            pass
                    if b['kb'] == b['nkb'] - 1 and b['qt'] == NQ2 - 1 and b['h'] + 2 < H:
                        hload(b['h'] + 2)
                for (cnt, bb) in pend:
                    epi2(bb)
                P.emit("ph3")
            return b_out

        mid = ExitStack()
        top.enter_context(mid)
        wo = sb(mid, "wo", [128, KC, D], BF16)
        b_wo = Buf("wo")
        b_scr3 = phase3() if nph >= 3 else None

        def phase4():
            with ExitStack() as st:
                g2 = sb(st, "g2", [128, D], F32)
                brt = sb(st, "brt", [128, 36], F32)
                mT = [sb(st, "mT%d" % i, [128, KC, 512], BF16) for i in range(2)]
                xt = [sb(st, "x4_%d" % i, [128, D], F32) for i in range(2)]
                hs = [sb(st, "hs%d" % i, [128, D], F32) for i in range(2)]
                xn2 = sb(st, "xn2", [128, D], F32)
                xn2b = [sb(st, "xn2b%d" % i, [128, D], BF16) for i in range(2)]
                jk = sb(st, "jk4", [128, D], BF16)
                xT32 = sb(st, "xT32", [128, KC, 128], F32)
                rs = [sb(st, "rs%d" % i, [128, 160], F32) for i in range(2)]
                cum = [sb(st, "cum%d" % i, [128, 32], F32) for i in range(2)]
                po = [ps(st, "po%d" % i, [128, 512]) for i in range(4)]
                ptr = ps(st, "ptr", [128, 1024])
                prt = ps(st, "prt", [128, 512])
                ppre = ps(st, "ppre", [128, 512])
                b_g2 = Buf("g2")
                b_mT = [Buf("mT0"), Buf("mT1")]
                b_xt = [Buf("x40"), Buf("x41")]
                b_hs = [Buf("hs0"), Buf("hs1")]
                b_xn2, b_jk, b_xT32 = Buf("xn2"), Buf("jk4"), Buf("xT32")
                b_xn2b = [Buf("xn2b0"), Buf("xn2b1")]
                b_rs = [Buf("rs0"), Buf("rs1")]
                b_cum = [Buf("cum0"), Buf("cum1")]
                b_po = [Buf("po%d" % i) for i in range(4)]
                b_ptr, b_prt, b_ppre = Buf("ptr"), Buf("prt"), Buf("ppre")
                b_H, b_XS = Buf("HH"), Buf("XS")
                P.dma('sp', g2[:, :], bc_d[:, 0:D], w=[b_g2], key="g2")
                P.dma('sp', brt[:, :], bc_d[:, 2 * D:2 * D + 36], ww=[b_g2], key="g2")
                P.add('dve', lambda e: e.memset(cum[0][:, :], 0.0), w=[b_cum[0]])
                MIXv = MIXT.ap().rearrange("(k p) t -> p k t", p=128)

                def mload(tq):
                    P.dma('sp', mT[tq % 2][:, :, :], MIXv[:, :, tq * 512:(tq + 1) * 512], r=[b_scr2, b_scr3], w=[b_mT[tq % 2]], key=("mT", tq % 2))

                def xload(i):
                    P.dma('sp', xt[i % 2][:, :], x[i * 128:(i + 1) * 128, :], w=[b_xt[i % 2]], key=("x4", i % 2))

                def mm_tile(i):
                    tq, sub = i // 4, i % 4
                    m, bm = mT[tq % 2], b_mT[tq % 2]
                    for cbk in range(4):
                        for k in range(KC):
                            P.mm(po[cbk][:, :], m[:, k, sub * 128:(sub + 1) * 128], wo[:, k, cbk * 512:(cbk + 1) * 512], k == 0, k == KC - 1,
                                 r=[bm, b_wo], w=[b_po[cbk]])

                def adds(i):
                    h_, bh = hs[i % 2], b_hs[i % 2]
                    for cbk in range(4):
                        P.tt('dve', h_[:, cbk * 512:(cbk + 1) * 512], po[cbk][:, :], xt[i % 2][:, cbk * 512:(cbk + 1) * 512], ALU.add,
                             r=[b_po[cbk], b_xt[i % 2]], w=[bh] if cbk == 0 else [], ww=[] if cbk == 0 else [bh])
                    P.dma('sp', HH[i * 128:(i + 1) * 128, :], h_[:, :], r=[bh], ww=[b_H], key=("hs", i % 2))

                def part1(i):
                    h_, bh = hs[i % 2], b_hs[i % 2]
                    R, bR = rs[i % 2], b_rs[i % 2]
                    P.ttr(jk[:, :], h_[:, :], h_[:, :], R[:, 0:1], r=[bh], w=[b_jk, bR])
                    P.ts('dve', R[:, 1:2], R[:, 0:1], 1.0 / D, EPS, ALU.mult, ALU.add, r=[bR], w=[bR])
                    P.tt('pool', R[:, 2:3], R[:, 1:2], mhalf, ALU.pow, r=[bR, b_sm], w=[bR])
                    P.stt(xn2[:, :], h_[:, :], R[:, 2:3], g2[:, :], ALU.mult, ALU.mult, r=[bh, bR, b_g2], w=[b_xn2])
                    P.cp('act', xn2b[i % 2][:, :], xn2[:, :], r=[b_xn2], w=[b_xn2b[i % 2]])

                def part2(i):
                    R, bR = rs[i % 2], b_rs[i % 2]
                    for hb in range(2):
                        for j in range(8):
                            k = hb * 8 + j
                            P.tr(ptr[:, j * 128:(j + 1) * 128], xn2[:, k * 128:(k + 1) * 128], ident, r=[b_xn2, b_const], w=[b_ptr])
                        P.cp('act' if hb == 0 else 'dve', xT32[:, hb * 8:(hb + 1) * 8, :], ptr[:, :].rearrange("p (k t) -> p k t", t=128),
                             r=[b_ptr], w=[b_xT32] if hb == 0 else [], ww=[] if hb == 0 else [b_xT32])
                    for k in range(KC):
                        P.mm(prt[:, 0:36], xT32[:, k, :], wr[:, k * 36:(k + 1) * 36], k == 0, k == KC - 1, r=[b_xT32, b_const], w=[b_prt])
                    lg = R[:, 4:40]
                    P.tt('dve', lg, prt[:, 0:36], brt[:, :], ALU.add, r=[b_prt, b_g2], w=[bR])
                    P.add('dve', lambda e, R=R: e.reduce_max(R[:, 40:41], R[:, 4:8], AX.X), r=[bR], w=[bR])
                    P.ts('dve', R[:, 41:42], R[:, 40:41], -1.0, None, ALU.mult, r=[bR], w=[bR])
                    P.act(R[:, 44:48], R[:, 4:8], AF.Exp, r=[bR], w=[bR], bias=R[:, 41:42], accum_out=R[:, 42:43])
                    P.add('dve', lambda e, R=R: e.reciprocal(R[:, 43:44], R[:, 42:43]), r=[bR], w=[bR])
                    P.ts('dve', R[:, 48:52], R[:, 4:8], R[:, 40:41], None, ALU.is_equal, r=[bR], w=[bR])
                    goh_b = bass.AP(R, R[:, 48:52].offset, [list(R[:, 48:52].ap[0]), [1, 4], [0, 8]])
                    P.ts('dve', R[:, 52:84].rearrange("p (g e) -> p g e", e=8), goh_b, 1e30, -1e30, ALU.mult, ALU.add, r=[bR], w=[bR])
                    P.tt('dve', R[:, 84:116], R[:, 8:40], R[:, 52:84], ALU.add, r=[bR], w=[bR])
                    P.add('dve', lambda e, R=R: e.max(R[:, 116:124], R[:, 84:116]), r=[bR], w=[bR])
                    P.ts('dve', R[:, 52:84], R[:, 84:116], R[:, 116:117], None, ALU.is_equal, r=[bR], w=[bR])
                    P.ts('dve', R[:, 124:156], R[:, 84:116], R[:, 117:118], None, ALU.is_equal, r=[bR], w=[bR])
                    P.tt('dve', R[:, 156:157], R[:, 117:118], R[:, 116:117], ALU.subtract, r=[bR], w=[bR])
                    P.act(R[:, 157:158], R[:, 156:157], AF.Exp, r=[bR], w=[bR])
                    P.ts('dve', R[:, 158:159], R[:, 157:158], 1.0, None, ALU.add, r=[bR], w=[bR])
                    P.add('dve', lambda e, R=R: e.reciprocal(R[:, 159:160], R[:, 158:159]), r=[bR], w=[bR])
                    P.tt('dve', rt[:, i:i + 1], R[:, 159:160], R[:, 43:44], ALU.mult, r=[bR], w=[b_rt])
                    P.tt('dve', rt[:, NT + i:NT + i + 1], R[:, 157:158], rt[:, i:i + 1], ALU.mult, r=[bR, b_rt], w=[b_rt])
                    P.tt('dve', R[:, 84:116], R[:, 52:84], R[:, 124:156], ALU.add, r=[bR], w=[bR])

                def part3(i):
                    R, bR = rs[i % 2], b_rs[i % 2]
                    P.mm(ppre[:, 0:32], triU, R[:, 84:116], True, True, r=[bR, b_const], w=[b_ppre])
                    P.mm(ppre[:, 32:64], ones_f, R[:, 84:116], True, True, r=[bR, b_const], w=[b_ppre])
                    c0, c1 = cum[i % 2], cum[(i + 1) % 2]
                    P.tt('dve', R[:, 84:116], ppre[:, 0:32], c0[:, :], ALU.add, r=[b_ppre, b_cum[i % 2]], w=[bR])
                    P.tt('dve', c1[:, :], ppre[:, 32:64], c0[:, :], ALU.add, r=[b_ppre, b_cum[i % 2]], w=[b_cum[(i + 1) % 2]])
                    P.ts('dve', R[:, 84:116], R[:, 84:116], float(CAP - 1), None, ALU.min, r=[bR], w=[bR])
                    P.tt('dve', R[:, 84:116], R[:, 84:116], ebase, ALU.add, r=[bR, b_const], w=[bR])
                    P.stt(R[:, 4:36], R[:, 52:84], 1.0, R[:, 84:116], ALU.mult, ALU.mult, r=[bR], w=[bR], accum_out=R[:, 36:37])
                    P.stt(R[:, 4:36], R[:, 124:156], 1.0, R[:, 84:116], ALU.mult, ALU.mult, r=[bR], w=[bR], accum_out=R[:, 37:38])
                    P.cp('dve', rti[:, i:i + 1], R[:, 36:37], r=[bR], w=[b_rt])
                    P.cp('dve', rti[:, NT + i:NT + i + 1], R[:, 37:38], r=[bR], w=[b_rt])
                    P.cp('dve', rt[:, 2 * NT + i:2 * NT + i + 1], R[:, 36:37], r=[bR], w=[b_rt])
                    P.cp('dve', rt[:, 3 * NT + i:3 * NT + i + 1], R[:, 37:38], r=[bR], w=[b_rt])
                    for kk in range(2):
                        ia = rti[:, kk * NT + i:kk * NT + i + 1]
                        P.add('pool', lambda e, ia=ia, src=xn2b[i % 2]: e.indirect_dma_start(
                            out=XS[:, :], out_offset=bass.IndirectOffsetOnAxis(ap=ia, axis=0), in_=src[:, :], in_offset=None),
                            r=[b_xn2b[i % 2], b_rt], ww=[b_XS], dkey=("sc", i % 2, kk))

                mload(0)
                xload(0)
                if NQ > 1:
                    mload(1)
                mm_tile(0)
                adds(0)
                for i in range(NT):
                    tq, sub = i // 4, i % 4
                    if sub == 3 and tq + 2 < NQ:
                        mload(tq + 2)
                    if i + 1 < NT:
                        xload(i + 1)
                    part1(i)
                    if i + 1 < NT:
                        mm_tile(i + 1)
                        adds(i + 1)
                    if i >= 1:
                        part3(i - 1)
                    part2(i)
                part3(NT - 1)
                if dbg:
                    P.dma('sp', RT[:, :], rt[:, :], r=[b_rt], w=[Buf("RTd")], key="rtd")
                P.emit("ph4")
            return b_H, b_XS

        if nph >= 4:
            b_H, b_XS = phase4()
        mid.close()

        def phase5():
            with ExitStack() as st:
                NR = 32
                ring = [sb(st, "ring%d" % i, [128, 2048], BF16) for i in range(NR)]
                NXB = 2 * NST
                xsb = [sb(st, "xsb%d" % i, [128, D], BF16) for i in range(NXB)]
                xT = sb(st, "xT5", [128, KC, CAP], BF16)
                hT = sb(st, "hT5", [128, 8, CAP], BF16)
                sgt = [sb(st, "sg5_%d" % i, [128, CAP], F32) for i in range(2)]
                ystg = [sb(st, "ystg%d" % i, [128, D], F32) for i in range(2)]
                ptx = ps(st, "ptx", [128, 2048], BF16)
                pg = [ps(st, "pg%d" % i, [128, 512]) for i in range(2)]
                pu = [ps(st, "pu%d" % i, [128, 512]) for i in range(2)]
                pd = [ps(st, "pd%d" % i, [128, 512]) for i in range(2)]
                b_ring = [Buf("ring%d" % i) for i in range(NR)]
                b_xsb = [Buf("xsb%d" % i) for i in range(NXB)]
                b_xT = [Buf("xT5_%d" % i) for i in range(NST)]
                b_hT = [Buf("hT5_%d" % i) for i in range(8)]
                b_sgt = [Buf("sg50"), Buf("sg51")]
                b_ystg = [Buf("ystg0"), Buf("ystg1")]
                b_ptx = Buf("ptx")
                b_pg, b_pu, b_pd = [Buf("pg0"), Buf("pg1")], [Buf("pu0"), Buf("pu1")], [Buf("pd0"), Buf("pd1")]
                b_YS = Buf("YS")
                cnt = {'r': 0, 'x': 0, 'pd': 0, 'y': 0, 'ev': 0}

                def wloads(e):
                    sg, sd = [], []
                    for k in range(KC):
                        s_ = cnt['r'] % NR
                        cnt['r'] += 1
                        P.dma('pool', ring[s_][:, 0:1024], w_gate[e, k * 128:(k + 1) * 128, :], w=[b_ring[s_]], key=("ring", s_))
                        P.dma('pool', ring[s_][:, 1024:2048], w_up[e, k * 128:(k + 1) * 128, :], ww=[b_ring[s_]], key=("ring", s_))
                        sg.append(s_)
                    for k in range(8):
                        s_ = cnt['r'] % NR
                        cnt['r'] += 1
                        P.dma('pool', ring[s_][:, :], w_down[e, k * 128:(k + 1) * 128, :], w=[b_ring[s_]], key=("ring", s_))
                        sd.append(s_)
                    return sg, sd

                def xloads(e):
                    ids = []
                    for s3 in range(NST):
                        xi = cnt['x'] % NXB
                        cnt['x'] += 1
                        r0 = e * CAP + s3 * 128
                        P.dma('sp', xsb[xi][:, :], XS[r0:r0 + 128, :], r=[b_XS], w=[b_xsb[xi]], key=("xsb", xi))
                        ids.append(xi)
                    return ids

                nw = wloads(0)
                nx = xloads(0)
                for e in range(NE):
                    (sg, sd), xids = nw, nx
                    if e + 1 < NE:
                        nx = xloads(e + 1)
                    for s3 in range(NST):
                        xi = xids[s3]
                        for k in range(KC):
                            P.tr(ptx[:, k * 128:(k + 1) * 128], xsb[xi][:, k * 128:(k + 1) * 128], ident_b, r=[b_xsb[xi], b_const], w=[b_ptx])
                        for hb in range(2):
                            P.cp('act' if hb == 0 else 'dve', xT[:, hb * 8:(hb + 1) * 8, s3 * 128:(s3 + 1) * 128],
                                 ptx[:, hb * 1024:(hb + 1) * 1024].rearrange("p (k t) -> p k t", t=128),
                                 r=[b_ptx], w=[b_xT[s3]] if hb == 0 else [], ww=[] if hb == 0 else [b_xT[s3]])
                    for fe in range(8):
                        pi = fe % 2
                        for k in range(KC):
                            P.mm(pg[pi][:, 0:CAP], ring[sg[k]][:, fe * 128:(fe + 1) * 128], xT[:, k, :], k == 0, k == KC - 1,
                                 r=[b_ring[sg[k]]] + b_xT, w=[b_pg[pi]])
                        for k in range(KC):
                            P.mm(pu[pi][:, 0:CAP], ring[sg[k]][:, 1024 + fe * 128:1024 + (fe + 1) * 128], xT[:, k, :], k == 0, k == KC - 1,
                                 r=[b_ring[sg[k]]] + b_xT, w=[b_pu[pi]])
                        P.act(sgt[pi][:, :], pg[pi][:, 0:CAP], AF.Silu, r=[b_pg[pi]], w=[b_sgt[pi]])
                        P.tt('dve', hT[:, fe, :], sgt[pi][:, :], pu[pi][:, 0:CAP], ALU.mult, r=[b_sgt[pi], b_pu[pi]], w=[b_hT[fe]])
                    if e + 1 < NE:
                        nw = wloads(e + 1)
                    for s3 in range(NST):
                        yi = cnt['y'] % 2
                        cnt['y'] += 1
                        for cbk in range(4):
                            pi = cnt['pd'] % 2
                            cnt['pd'] += 1
                            for k in range(8):
                                P.mm(pd[pi][:, :], hT[:, k, s3 * 128:(s3 + 1) * 128], ring[sd[k]][:, cbk * 512:(cbk + 1) * 512], k == 0, k == 7,
                                     r=[b_hT[k], b_ring[sd[k]]], w=[b_pd[pi]])
                            cnt['ev'] += 1
                            P.cp('act' if cnt['ev'] % 2 else 'dve', ystg[yi][:, cbk * 512:(cbk + 1) * 512], pd[pi][:, :],
                                 r=[b_pd[pi]], w=[b_ystg[yi]] if cbk == 0 else [], ww=[] if cbk == 0 else [b_ystg[yi]])
                        r0 = e * CAP + s3 * 128
                        P.dma('sp', YS[r0:r0 + 128, :], ystg[yi][:, :], r=[b_ystg[yi]], ww=[b_YS], key=("ystg", yi))
                P.emit("ph5")
            return b_YS

        if nph >= 5:
            b_YS = phase5()

        def phase6():
            with ExitStack() as st:
                gf = sb(st, "gf", [128, D], F32)
                hb_ = [sb(st, "h6_%d" % i, [128, D], F32) for i in range(2)]
                y1 = [sb(st, "y1_%d" % i, [128, D], F32) for i in range(2)]
                y2 = [sb(st, "y2_%d" % i, [128, D], F32) for i in range(2)]
                ob = [sb(st, "ob%d" % i, [128, D], F32) for i in range(2)]
                jk = sb(st, "jk6", [128, D], BF16)
                s6 = [sb(st, "s6_%d" % i, [128, 4], F32) for i in range(2)]
                b_gf = Buf("gf")
                b_h = [Buf("h60"), Buf("h61")]
                b_y1 = [Buf("y10"), Buf("y11")]
                b_y2 = [Buf("y20"), Buf("y21")]
                b_ob = [Buf("ob0"), Buf("ob1")]
                b_jk = Buf("jk6")
                b_s6 = [Buf("s60"), Buf("s61")]
                b_fin = Buf("fin")
                P.dma('sp', gf[:, :], bc_d[:, D:2 * D], w=[b_gf], key="gf")

                def loads(i):
                    a = i % 2
                    P.dma('sp', hb_[a][:, :], HH[i * 128:(i + 1) * 128, :], r=[b_H], w=[b_h[a]], key=("h6", a))
                    for kk, (yy, by) in enumerate(((y1, b_y1), (y2, b_y2))):
                        ia = rti[:, kk * NT + i:kk * NT + i + 1]
                        P.add('pool', lambda e, ia=ia, dst=yy[a]: e.indirect_dma_start(
                            out=dst[:, :], out_offset=None, in_=YS[:, :], in_offset=bass.IndirectOffsetOnAxis(ap=ia, axis=0)),
                            r=[b_YS, b_rt], w=[by[a]], dkey=("ga", a, kk))
                loads(0)
                for i in range(NT):
                    if i + 1 < NT:
                        loads(i + 1)
                    a = i % 2
                    P.stt(hb_[a][:, :], y1[a][:, :], rt[:, i:i + 1], hb_[a][:, :], ALU.mult, ALU.add, r=[b_y1[a], b_rt], w=[b_h[a]])
                    P.stt(hb_[a][:, :], y2[a][:, :], rt[:, NT + i:NT + i + 1], hb_[a][:, :], ALU.mult, ALU.add, r=[b_y2[a], b_rt], w=[b_h[a]])
                    P.act(jk[:, :], hb_[a][:, :], AF.Square, r=[b_h[a]], w=[b_jk, b_s6[a]], accum_out=s6[a][:, 0:1])
                    P.ts('dve', s6[a][:, 1:2], s6[a][:, 0:1], 1.0 / D, EPS, ALU.mult, ALU.add, r=[b_s6[a]], w=[b_s6[a]])
                    P.tt('pool', s6[a][:, 2:3], s6[a][:, 1:2], mhalf, ALU.pow, r=[b_s6[a], b_sm], w=[b_s6[a]])
                    P.stt(ob[a][:, :], hb_[a][:, :], s6[a][:, 2:3], gf[:, :], ALU.mult, ALU.mult, r=[b_h[a], b_s6[a], b_gf], w=[b_ob[a]])
                    P.dma('sp', out_d[i * 128:(i + 1) * 128, :], ob[a][:, :], r=[b_ob[a]], ww=[b_fin], key=("ob", a))
                P.add('sp', lambda e: e.nop(), r=[b_fin])
                P.emit("ph6")

        if nph >= 6:
            phase6()
    return nc


def make_consts(S, CAP):
    NT = S // 128
    ND = NT + 3
    k = np.arange(128)
    cf = np.zeros((128, 416 + 8 * ND), np.float32)
    cf[:, 0:128] = np.eye(128, dtype=np.float32)
    cf[:, 128:256] = (k[:, None] < k[None, :]).astype(np.float32)
    cf[:, 256:384] = 1.0
    cf[:, 384:416] = (np.arange(32) * CAP)[None, :]
    sl = _slopes()
    for h in range(H):
        for di in range(ND):
            cf[:, 416 + h * ND + di] = sl[h] * (k - 128.0 * (di - 3))
    cb = np.zeros((128, 512), np.float32)
    cb[:, 384:512] = np.where(k[:, None] > k[None, :], -30000.0, 0.0)
    cb[:, 0:128] = np.eye(128)
    cb[:, 128:256] = 1.0
    cb[:, 256:384] = (k[:, None] <= k[None, :])
    cb = cb.astype(ml_dtypes.bfloat16)
    qi = np.arange(S) % 256
    lo = qi % 256
    qaug = np.stack([np.stack([-8.0 * sl[h] * lo, -8.0 * sl[h] * (qi - lo)]) for h in range(H)]).astype(np.float32).astype(ml_dtypes.bfloat16)
    return cf, cb, qaug


def make_params(inp):
    f = lambda a: np.asarray(a, np.float32)
    pp = np.zeros((128, 16 + 8 * TAPS + 24 + 1 + 256), np.float32)
    pp[:, 0:16] = f(inp['norm_mix_g'])[0].reshape(16, 128).T
    cw = f(inp['conv_w'])[0, :, 0, :]
    pp[:, 16:16 + 8 * TAPS] = cw.reshape(TAPS, 8, 128).transpose(2, 1, 0).reshape(128, 8 * TAPS)
    o = 16 + 8 * TAPS
    pp[:, o:o + 8] = f(inp['conv_b'])[0].reshape(8, 128).T
    pp[:, o + 8:o + 16] = f(inp['conv_ln_g'])[0].reshape(8, 128).T
    pp[:, o + 16:o + 24] = f(inp['conv_ln_b'])[0].reshape(8, 128).T
    pp[:, o + 24] = f(inp['subln_g'])[0]
    lam = np.concatenate([f(inp['lambda_q1'])[0], f(inp['lambda_k1'])[0], f(inp['lambda_q2'])[0], f(inp['lambda_k2'])[0]])
    pp[:, o + 25:o + 25 + 256] = lam[None, :]
    bc = np.zeros((128, 2 * D + 36), np.float32)
    bc[:, 0:D] = f(inp['norm_ffn_g'])[0][None, :]
    bc[:, D:2 * D] = f(inp['norm_final_g'])[None, :]
    bc[:, 2 * D:2 * D + 4] = f(inp['b_group_router'])[0][None, :]
    bc[:, 2 * D + 4:] = f(inp['b_expert_router'])[0][None, :]
    wrc = np.concatenate([f(inp['w_group_router'])[0], f(inp['w_expert_router'])[0]], axis=1)
    wr = np.ascontiguousarray(wrc.reshape(16, 128, 36).transpose(1, 0, 2).reshape(128, 16 * 36))
    return pp, bc, wr


def make_in_maps(inp, S, CAP, cores):
    cf, cb, qaug = make_consts(S, CAP)
    pp, bc, wr = make_params(inp)
    f = lambda a: np.ascontiguousarray(np.asarray(a, np.float32))
    shared = dict(w_in=f(inp['w_in'])[0], w_out=f(inp['w_out'])[0], w_gate=f(inp['w_gate'])[0], w_up=f(inp['w_up'])[0],
                  w_down=f(inp['w_down'])[0], cf=cf, cb=cb, qaug=qaug, pp=pp, bc=bc, wr=wr)
    xs = np.asarray(inp['x'], np.float32)
    maps = []
    for b in cores:
        m = dict(shared)
        m['x'] = np.ascontiguousarray(xs[b, :S])
        maps.append(m)
    return maps


_NC_CACHE = {}


def kernel(**inputs):
    S, CAP = 4096, 384
    if 'nc' not in _NC_CACHE:
        _NC_CACHE['nc'] = build(S, CAP)
    nc = _NC_CACHE['nc']
    maps = make_in_maps(inputs, S, CAP, list(range(8)))
    res = run_bass_kernel_spmd(nc, maps, core_ids=list(range(8)))
    return np.stack([np.asarray(r["out"], np.float32) for r in res.results], axis=0)
```

```python
import math
from contextlib import ExitStack
import numpy as np
import ml_dtypes
import concourse.bass as bass
import concourse.mybir as mybir
from concourse.bass_utils import run_bass_kernel_spmd

F32 = mybir.dt.float32
BF16 = mybir.dt.bfloat16
I32 = mybir.dt.int32
AF = mybir.ActivationFunctionType
ALU = mybir.AluOpType
AX = mybir.AxisListType

D = 2048
KC = 16
H = 8
NE = 32
EPS = 1e-6
TAPS = 31
ENG = ['pe', 'act', 'dve', 'pool', 'sp']
SAME_ENGINE_SYNC = True


class Buf:
    __slots__ = ('name', 'wc', 'wd', 'rc', 'rd')

    def __init__(self, name):
        self.name = name
        self.wc = {}
        self.wd = {}
        self.rc = {}
        self.rd = {}


class Op:
    __slots__ = ('eng', 'fn', 'cdeps', 'ddeps', 'sig', 'sigval', 'dma', 'emitted')


class Prog:
    def __init__(self, nc, stack):
        self.nc = nc
        self.stack = stack
        self.ops = {e: [] for e in ENG}
        self.csem = {e: stack.enter_context(nc.semaphore("cs_" + e)) for e in ENG}
        self.ccount = {e: 0 for e in ENG}
        self.waited = {e: {} for e in ENG}
        self.dsems = {}
        self.nsem = len(ENG)

    def dsem(self, key):
        ent = self.dsems.get(key)
        if ent is None:
            ent = [self.stack.enter_context(self.nc.semaphore("ds%d" % len(self.dsems))), 0]
            self.dsems[key] = ent
            self.nsem += 1
        return ent

    def add(self, eng, fn, r=(), w=(), dkey=None, ww=()):
        op = Op()
        op.eng, op.fn, op.cdeps, op.ddeps = eng, fn, [], []
        op.sig, op.sigval, op.dma, op.emitted = False, None, None, False
        if dkey is not None:
            ent = self.dsem(dkey)
            ent[1] += 16
            op.dma = (ent[0], ent[1])

        def cdep(d):
            if d is op:
                return
            if d.eng != eng or op.dma is not None or (SAME_ENGINE_SYNC and eng != 'pe'):
                op.cdeps.append(d)
                if not d.emitted:
                    d.sig = True

        for b in r:
            for d in b.wc.values():
                cdep(d)
            for sv in b.wd.items():
                op.ddeps.append(sv)
        for b in w:
            for d in b.wc.values():
                cdep(d)
            for d in b.rc.values():
                cdep(d)
            for sv in b.wd.items():
                op.ddeps.append(sv)
            for sv in b.rd.items():
                op.ddeps.append(sv)
        for b in r:
            if b in w:
                continue
            if op.dma is not None:
                b.rd[op.dma[0]] = op.dma[1]
            else:
                b.rc[eng] = op
        for b in w:
            b.rc.clear()
            b.rd.clear()
            b.wc.clear()
            b.wd.clear()
            if op.dma is not None:
                b.wd[op.dma[0]] = op.dma[1]
            else:
                b.wc[eng] = op
        for b in ww:
            if op.dma is not None:
                b.wd[op.dma[0]] = op.dma[1]
            else:
                b.wc[eng] = op
        self.ops[eng].append(op)
        return op

    def emit(self, name):
        nc = self.nc
        for e in ENG:
            for op in self.ops[e]:
                if op.sig and op.dma is None:
                    self.ccount[e] += 1
                    op.sigval = self.ccount[e]
        endval = {}
        for e in ENG:
            self.ccount[e] += 1
            endval[e] = self.ccount[e]
        ops, csem, waited = self.ops, self.csem, self.waited

        def run(e, eng):
            wt = waited[e]
            for op in ops[e]:
                for d in op.cdeps:
                    if d.sigval is None:
                        continue
                    s = csem[d.eng]
                    if wt.get(s, 0) < d.sigval:
                        eng.wait_ge(s, d.sigval)
                        wt[s] = d.sigval
                for (s, v) in op.ddeps:
                    if wt.get(s, 0) < v:
                        eng.wait_ge(s, v)
                        wt[s] = v
                ins = op.fn(eng)
                if op.dma is not None:
                    ins.then_inc(op.dma[0], 16)
                elif op.sig:
                    ins.then_inc(csem[e], 1)
                op.emitted = True
            eng.drain().then_inc(csem[e], 1)
            for e2 in ENG:
                if e2 != e:
                    eng.wait_ge(csem[e2], endval[e2])
                    wt[csem[e2]] = endval[e2]

        with nc.Block(name) as block:
            @block.tensor
            def _(eng):
                run('pe', eng)

            @block.scalar
            def _(eng):
                run('act', eng)

            @block.vector
            def _(eng):
                run('dve', eng)

            @block.gpsimd
            def _(eng):
                run('pool', eng)

            @block.sync
            def _(eng):
                run('sp', eng)
        self.ops = {e: [] for e in ENG}

    def mm(self, out, lhsT, rhs, start, stop, r=(), w=(), ww=()):
        return self.add('pe', lambda e: e.matmul(out, lhsT, rhs, start=start, stop=stop), r, w, ww=ww)

    def tr(self, out, in_, ident, r=(), w=()):
        return self.add('pe', lambda e: e.transpose(out, in_, ident), r, w)

    def act(self, out, in_, func, r=(), w=(), ww=(), **kw):
        return self.add('act', lambda e: e.activation(out, in_, func, **kw), r, w, ww=ww)

    def ts(self, eng, out, in0, s1, s2, op0, op1=None, r=(), w=(), ww=(), **kw):
        if op1 is None:
            return self.add(eng, lambda e: e.tensor_scalar(out, in0, s1, None, op0, **kw), r, w, ww=ww)
        return self.add(eng, lambda e: e.tensor_scalar(out, in0, s1, s2, op0, op1, **kw), r, w, ww=ww)

    def tt(self, eng, out, in0, in1, op, r=(), w=(), ww=()):
        return self.add(eng, lambda e: e.tensor_tensor(out, in0, in1, op), r, w, ww=ww)

    def stt(self, out, in0, scalar, in1, op0, op1, r=(), w=(), **kw):
        return self.add('dve', lambda e: e.scalar_tensor_tensor(out, in0, scalar, in1, op0, op1, **kw), r, w)

    def ttr(self, out, in0, in1, accum, r=(), w=()):
        return self.add('dve', lambda e: e.scalar_tensor_tensor(out, in0, 1.0, in1, ALU.mult, ALU.mult, accum_out=accum), r, w)

    def cp(self, eng, out, in_, r=(), w=(), ww=()):
        if eng == 'act':
            return self.add('act', lambda e: e.copy(out, in_), r, w, ww=ww)
        return self.add(eng, lambda e: e.tensor_copy(out, in_), r, w, ww=ww)

    def dma(self, q, out, in_, r=(), w=(), key=None, ww=(), **kw):
        return self.add(q, lambda e: e.dma_start(out, in_, **kw), r, w, dkey=key, ww=ww)


def _slopes():
    return [2.0 ** (-8.0 * (i + 1) / H) for i in range(H)]


def build(S=4096, CAP=384, dbg=False, nph=99):
    NT = S // 128
    NQ = S // 512
    NHALF = max(1, S // 2048)
    HS = S // NHALF
    ND = NT + 3
    NST = CAP // 128
    nc = bass.Bass("TRN2", target_bir_lowering=False)

    def din(name, shape, dt=F32):
        return nc.dram_tensor(name, list(shape), dt, kind="ExternalInput")

    def dscr(name, shape, dt):
        return nc.dram_tensor(name, list(shape), dt, kind="ExternalOutput" if dbg else "Internal")

    x = din("x", [S, D])
    w_in = din("w_in", [D, 5120])
    w_out = din("w_out", [D, D])
    w_gate = din("w_gate", [NE, D, 1024])
    w_up = din("w_up", [NE, D, 1024])
    w_down = din("w_down", [NE, 1024, D])
    cf_d = din("cf", [128, 416 + 8 * ND])
    cb_d = din("cb", [128, 512], BF16)
    qaug_d = din("qaug", [H, 2, S], BF16)
    pp_d = din("pp", [128, 16 + 8 * TAPS + 24 + 1 + 256])
    bc_d = din("bc", [128, 2 * D + 36])
    wr_d = din("wr", [128, KC * 36])
    out_d = nc.dram_tensor("out", [S, D], F32, kind="ExternalOutput")

    QT = dscr("QT", [H, 128, S], BF16)
    KT = dscr("KT", [H, 128, S], BF16)
    VV = dscr("VV", [S, 1024], BF16)
    UT = dscr("UT", [1024, S], BF16)
    MIXT = dscr("MIXT", [D, S], BF16)
    HH = dscr("HH", [S, D], F32)
    XS = dscr("XS", [NE * CAP, D], BF16)
    YS = dscr("YS", [NE * CAP, D], F32)
    RT = dscr("RT", [128, 4 * NT], F32)

    top = ExitStack()
    with top:
        P = Prog(nc, top)

        def sb(stack, name, shape, dt):
            return stack.enter_context(nc.sbuf_tensor("s_" + name, list(shape), dt))

        def ps(stack, name, shape, dt=F32):
            return stack.enter_context(nc.psum_tensor("p_" + name, list(shape), dt))

        cf = sb(top, "cf", [128, 416 + 8 * ND], F32)
        cb = sb(top, "cb", [128, 512], BF16)
        pp = sb(top, "pp", [128, 16 + 8 * TAPS + 24 + 1 + 256], F32)
        wr = sb(top, "wr", [128, KC * 36], F32)
        rt = sb(top, "rt", [128, 4 * NT], F32)
        rti = sb(top, "rti", [128, 2 * NT], I32)
        sm = sb(top, "sm", [128, 16], F32)
        b_const = Buf("const")
        b_rt = Buf("rt")
        ident = cf[:, 0:128]
        triU = cf[:, 128:256]
        ones_f = cf[:, 256:384]
        ebase = cf[:, 384:416]
        ALIB = 416
        ident_b = cb[:, 0:128]
        ones_b = cb[:, 128:256]
        tri_b = cb[:, 256:384]
        negtri_b = cb[:, 384:512]
        PP_G = 0
        PP_CW = 16
        PP_CB = PP_CW + 8 * TAPS
        PP_LG = PP_CB + 8
        PP_LB = PP_LG + 8
        PP_SG = PP_LB + 8
        PP_LAM = PP_SG + 1
        neglam = sm[:, 0:1]
        g08 = sm[:, 1:2]
        mhalf = sm[:, 2:3]

        b_sm = Buf("sm")
        P.dma('sp', cf[:, :], cf_d[:, :], w=[b_const], key="c0")
        P.dma('sp', cb[:, :], cb_d[:, :], w=[b_const], key="c0")
        P.dma('sp', pp[:, :], pp_d[:, :], w=[b_const], key="c0")
        P.dma('sp', wr[:, :], wr_d[:, :], w=[b_const], key="c0")
        lq = PP_LAM
        P.add('dve', lambda e: e.memset(sm[:, :], 0.0), w=[b_sm])
        P.add('dve', lambda e: e.memset(mhalf, -0.5), w=[b_sm])
        with ExitStack() as st0:
            junk = sb(st0, "junk0", [128, 64], F32)
            b_j = Buf("junk0")
            P.ttr(junk[:, :], pp[:, lq:lq + 64], pp[:, lq + 64:lq + 128], sm[:, 4:5], r=[b_const], w=[b_sm, b_j])
            P.ttr(junk[:, :], pp[:, lq + 128:lq + 192], pp[:, lq + 192:lq + 256], sm[:, 5:6], r=[b_const], w=[b_sm, b_j])
            P.act(sm[:, 6:8], sm[:, 4:6], AF.Exp, r=[b_sm], w=[b_sm])
            P.tt('dve', sm[:, 3:4], sm[:, 7:8], sm[:, 6:7], ALU.subtract, r=[b_sm], w=[b_sm])
            P.ts('dve', neglam, sm[:, 3:4], -(0.8 - 0.6), None, ALU.add, r=[b_sm], w=[b_sm])
            P.ts('dve', g08, pp[:, PP_SG:PP_SG + 1], 1.0 - (0.8 - 0.6), None, ALU.mult, r=[b_sm, b_const], w=[b_sm])
            P.emit("ph0")

        def phase1():
            with ExitStack() as st:
                CS = min(1024, S)
                NCH = S // CS
                ntl = CS // 128
                xnT2 = [sb(st, "xnT%d" % i, [128, KC, CS], BF16) for i in range(2)]
                NW = 4
                Wb = [sb(st, "Wb%d" % i, [128, KC, 512], BF16) for i in range(NW)]
                stg = [sb(st, "stg%d" % i, [128, CS], BF16) for i in range(3)]
                xt = [sb(st, "xt%d" % i, [128, D], F32) for i in range(2)]
                jk = sb(st, "jk", [128, D], BF16)
                sgt = [sb(st, "sgt%d" % i, [128, 512], F32) for i in range(2)]
                st1 = sb(st, "st1", [128, 8], F32)
                tp = [ps(st, "tp%d" % i, [128, 512]) for i in range(2)]
                pj = [ps(st, "pj%d" % i, [128, 512]) for i in range(4)]
                b_xnT2 = [[Buf("xnT%d_%d" % (u, i)) for i in range(ntl)] for u in range(2)]
                b_W = [Buf("W%d" % i) for i in range(NW)]
                b_stg = [Buf("stg%d" % i) for i in range(3)]
                b_xt = [Buf("xt%d" % i) for i in range(2)]
                b_jk = Buf("jk")
                b_sgt = [Buf("sgt%d" % i) for i in range(2)]
                b_st1 = [Buf("st1a"), Buf("st1b")]
                b_tp = [Buf("tp0"), Buf("tp1")]
                b_pj = [Buf("pj%d" % i) for i in range(4)]
                b_scr = Buf("scr1")
                cnt = {'w': 0, 'stg': 0, 'pj': 0, 'ev': 0}

                def evac(out, in_, r, w):
                    cnt['ev'] += 1
                    if cnt['ev'] % 2:
                        P.cp('act', out, in_, r=r, w=w)
                    else:
                        P.cp('dve', out, in_, r=r, w=w)

                def wload(c0):
                    s = cnt['w'] % NW
                    cnt['w'] += 1
                    src = w_in[:, c0:c0 + 512].rearrange("(k p) c -> p k c", p=128)
                    for kq in range(4):
                        P.dma('pool', Wb[s][:, kq * 4:(kq + 1) * 4, :], src[:, kq * 4:(kq + 1) * 4, :], w=[b_W[s]], key=("W", s))
                    return s

                def xload(gt):
                    P.dma('sp', xt[gt % 2][:, :], x[gt * 128:(gt + 1) * 128, :], w=[b_xt[gt % 2]], key=("xt", gt % 2))

                def norm_pre(gt):
                    xs_, bx = xt[gt % 2], b_xt[gt % 2]
                    a = (gt % 2) * 4
                    bs = b_st1[gt % 2]
                    P.ttr(jk[:, :], xs_[:, :], xs_[:, :], st1[:, a:a + 1], r=[bx], w=[b_jk, bs])
                    P.ts('dve', st1[:, a + 1:a + 2], st1[:, a:a + 1], 1.0 / D, EPS, ALU.mult, ALU.add, r=[bs], w=[bs])
                    P.tt('pool', st1[:, a + 2:a + 3], st1[:, a + 1:a + 2], mhalf, ALU.pow, r=[bs, b_sm], w=[bs])
                    P.ts('dve', xs_[:, :], xs_[:, :], st1[:, a + 2:a + 3], None, ALU.mult, r=[bs, bx], w=[bx])

                def norm_tr(gt):
                    c, i = gt // ntl, gt % ntl
                    xnT, b_xnT = xnT2[c % 2], b_xnT2[c % 2]
                    xs_, bx = xt[gt % 2], b_xt[gt % 2]
                    for g4 in range(4):
                        tpi, btp = tp[g4 % 2], b_tp[g4 % 2]
                        for j in range(4):
                            k = g4 * 4 + j
                            P.tr(tpi[:, j * 128:(j + 1) * 128], xs_[:, k * 128:(k + 1) * 128], ident, r=[bx, b_const], w=[btp])
                        for j in range(4):
                            k = g4 * 4 + j
                            o = xnT[:, k, i * 128:(i + 1) * 128]
                            gcol = pp[:, PP_G + k:PP_G + k + 1]
                            if j % 2 == 0:
                                P.act(o, tpi[:, j * 128:(j + 1) * 128], AF.Copy, r=[btp, b_const], w=[b_xnT[i]], scale=gcol)
                            else:
                                P.ts('dve', o, tpi[:, j * 128:(j + 1) * 128], gcol, None, ALU.mult, r=[btp, b_const], w=[b_xnT[i]])

                def step(k):
                    norm_tr(k)
                    if k + 2 < NT:
                        xload(k + 2)
                    if k + 1 < NT:
                        norm_pre(k + 1)

                worder = []
                for c_ in range(NCH):
                    worder += [0, 512, 1024, 1536, 2048, 2560, 3072, 4096, 3584, 4608]
                wstate = {'issued': 0}

                def need(idx):
                    while wstate['issued'] < min(len(worder), idx + 3):
                        wload(worder[wstate['issued']])
                        wstate['issued'] += 1
                    return idx % NW

                xload(0)
                if NT > 1:
                    xload(1)
                norm_pre(0)
                for gt in range(ntl):
                    step(gt)
                for c in range(NCH):
                    t0 = c * CS
                    xnT, b_xnT = xnT2[c % 2], b_xnT2[c % 2]
                    nxt_tiles = list(range((c + 1) * ntl, (c + 2) * ntl)) if c + 1 < NCH else []

                    def interleave():
                        if nxt_tiles:
                            step(nxt_tiles.pop(0))

                    def feat_major(ws, sub, dst_ap):
                        si = cnt['stg'] % 3
                        cnt['stg'] += 1
                        for tq in range(CS // 512):
                            pi = cnt['pj'] % 4
                            cnt['pj'] += 1
                            for k in range(KC):
                                P.mm(pj[pi][:, :], Wb[ws][:, k, sub * 128:(sub + 1) * 128], xnT[:, k, tq * 512:(tq + 1) * 512],
                                     k == 0, k == KC - 1, r=[b_W[ws]] + b_xnT[tq * 4:tq * 4 + 4], w=[b_pj[pi]])
                            evac(stg[si][:, tq * 512:(tq + 1) * 512], pj[pi][:, :], r=[b_pj[pi]], w=[b_stg[si]])
                        P.dma('sp', dst_ap, stg[si][:, :], r=[b_stg[si]], w=[b_scr], key=("stg", si))

                    wb0 = c * 10
                    for g in range(4):
                        ws = need(wb0 + g)
                        for sub in range(4):
                            hh = (g % 2) * 4 + sub
                            dst = (QT if g < 2 else KT)[hh, :, t0:t0 + CS]
                            feat_major(ws, sub, dst)
                        interleave()
                    for g in range(2):
                        ws = need(wb0 + 4 + g)
                        for i in range(ntl):
                            pi = cnt['pj'] % 4
                            cnt['pj'] += 1
                            for k in range(KC):
                                P.mm(pj[pi][:, :], xnT[:, k, i * 128:(i + 1) * 128], Wb[ws][:, k, :], k == 0, k == KC - 1,
                                     r=[b_W[ws], b_xnT[i]], w=[b_pj[pi]])
                            si = cnt['stg'] % 3
                            cnt['stg'] += 1
                            evac(stg[si][:, 0:512], pj[pi][:, :], r=[b_pj[pi]], w=[b_stg[si]])
                            P.dma('sp', VV[t0 + i * 128:t0 + (i + 1) * 128, g * 512:(g + 1) * 512], stg[si][:, 0:512],
                                  r=[b_stg[si]], w=[b_scr], key=("stg", si))
                        interleave()
                    for g in range(2):
                        wa = need(wb0 + 6 + 2 * g)
                        wg = need(wb0 + 7 + 2 * g)
                        for sub in range(4):
                            si = cnt['stg'] % 3
                            cnt['stg'] += 1
                            for tq in range(CS // 512):
                                pa = cnt['pj'] % 4
                                pg = (cnt['pj'] + 1) % 4
                                cnt['pj'] += 2
                                rr_ = b_xnT[tq * 4:tq * 4 + 4]
                                for k in range(KC):
                                    P.mm(pj[pa][:, :], Wb[wa][:, k, sub * 128:(sub + 1) * 128], xnT[:, k, tq * 512:(tq + 1) * 512],
                                         k == 0, k == KC - 1, r=[b_W[wa]] + rr_, w=[b_pj[pa]])
                                for k in range(KC):
                                    P.mm(pj[pg][:, :], Wb[wg][:, k, sub * 128:(sub + 1) * 128], xnT[:, k, tq * 512:(tq + 1) * 512],
                                         k == 0, k == KC - 1, r=[b_W[wg]] + rr_, w=[b_pj[pg]])
                                sgi = tq % 2
                                P.act(sgt[sgi][:, :], pj[pg][:, :], AF.Sigmoid, r=[b_pj[pg]], w=[b_sgt[sgi]])
                                P.tt('dve', stg[si][:, tq * 512:(tq + 1) * 512], pj[pa][:, :], sgt[sgi][:, :], ALU.mult,
                                     r=[b_pj[pa], b_sgt[sgi]], w=[b_stg[si]])
                            ch0 = g * 512 + sub * 128
                            P.dma('sp', UT[ch0:ch0 + 128, t0:t0 + CS], stg[si][:, :], r=[b_stg[si]], w=[b_scr], key=("stg", si))
                            if sub % 2 == 1:
                                interleave()
                    while nxt_tiles:
                        interleave()
                P.emit("ph1")
            return b_scr

        b_scr1 = phase1()

        def phase2():
            with ExitStack() as st:
                Dg = sb(st, "Dg", [128, 8 * TAPS, 128], BF16)
                U = [sb(st, "U%d" % i, [128, 8, 544], BF16) for i in range(2)]
                csb2 = [sb(st, "csb%d" % i, [128, 8, 512], F32) for i in range(2)]
                csq2 = [sb(st, "csq%d" % i, [128, 8, 512], F32) for i in range(2)]
                mean_sb = sb(st, "mean_sb", [128, 512], F32)
                msq = sb(st, "msq", [128, 512], F32)
                var = sb(st, "var", [128, 512], F32)
                rstd = sb(st, "rstd", [128, 512], F32)
                tmp = [sb(st, "tmp%d" % i, [128, 512], F32) for i in range(2)]
                tm2 = [sb(st, "tm2%d" % i, [128, 512], F32) for i in range(2)]
                ostg = [sb(st, "ostg%d" % i, [128, 512], BF16) for i in range(2)]
                pc = [ps(st, "pc%d" % i, [128, 512]) for i in range(2)]
                pst = [ps(st, "pst%d" % i, [128, 512]) for i in range(2)]
                b_Dg = [Buf("Dg%d" % i) for i in range(8)]
                b_U = [Buf("U0"), Buf("U1")]
                b_csb2 = [[Buf("csb%d_%d" % (u, i)) for i in range(8)] for u in range(2)]
                b_csq2 = [[Buf("csq%d_%d" % (u, i)) for i in range(8)] for u in range(2)]
                b_pc = [Buf("pc0"), Buf("pc1")]
                b_pst = [Buf("pst0"), Buf("pst1")]
                b_stat = Buf("stat")
                b_tmp = [Buf("tmp0"), Buf("tmp1")]
                b_tm2 = [Buf("tm20"), Buf("tm21")]
                b_ostg = [Buf("ostg0"), Buf("ostg1")]
                b_out = Buf("scr2")
                for cc in range(8):
                    for j in range(TAPS):
                        col = PP_CW + cc * TAPS + j
                        eng = 'dve'
                        P.ts(eng, Dg[:, cc * TAPS + j, :], ident, pp[:, col:col + 1], None, ALU.mult,
                             r=[b_const], ww=[b_Dg[cc]])
                UTv = UT.ap().rearrange("(c p) t -> p c t", p=128)

                def uload(tt):
                    u = tt % 2
                    if tt == 0:
                        P.add('pool', lambda e: e.memset(U[0][:, :, 0:30], 0.0), w=[b_U[0]])
                        P.dma('sp', U[0][:, :, 30:542], UTv[:, :, 0:512], r=[b_scr1], w=[b_U[0]], key=("U", 0))
                    else:
                        P.dma('sp', U[u][:, :, 0:542], UTv[:, :, tt * 512 - 30:tt * 512 + 512], r=[b_scr1], w=[b_U[u]], key=("U", u))
                uload(0)
                nn = {'n': 0}

                def ln_apply(tt, cc):
                    csb, b_csb = csb2[tt % 2], b_csb2[tt % 2]
                    ti = nn['n'] % 2
                    nn['n'] += 1
                    P.tt('dve', tmp[ti][:, :], csb[:, cc, :], mean_sb[:, :], ALU.subtract, r=[b_csb[cc], b_stat], w=[b_tmp[ti]])
                    P.tt('dve', tm2[ti][:, :], tmp[ti][:, :], rstd[:, :], ALU.mult, r=[b_tmp[ti], b_stat], w=[b_tm2[ti]])
                    P.act(ostg[ti][:, :], tm2[ti][:, :], AF.Silu, r=[b_tm2[ti], b_const], w=[b_ostg[ti]],
                          scale=pp[:, PP_LG + cc:PP_LG + cc + 1], bias=pp[:, PP_LB + cc:PP_LB + cc + 1])
                    P.dma('sp', MIXT[1024 + cc * 128:1024 + (cc + 1) * 128, tt * 512:(tt + 1) * 512], ostg[ti][:, :],
                          r=[b_ostg[ti]], ww=[b_out], key=("ostg", ti))

                for tt in range(NQ):
                    if tt + 1 < NQ:
                        uload(tt + 1)
                    u = tt % 2
                    csb, csq, b_csb, b_csq = csb2[u], csq2[u], b_csb2[u], b_csq2[u]
                    for cc in range(8):
                        pi = cc % 2
                        for j in range(TAPS):
                            P.mm(pc[pi][:, :], Dg[:, cc * TAPS + j, :], U[u][:, cc, j:j + 512], j == 0, j == TAPS - 1,
                                 r=[b_Dg[cc], b_U[u]], w=[b_pc[pi]])
                        cbias = pp[:, PP_CB + cc:PP_CB + cc + 1]
                        P.act(csb[:, cc, :], pc[pi][:, :], AF.Identity, r=[b_pc[pi], b_const], w=[b_csb[cc]], bias=cbias)
                        P.act(csq[:, cc, :], pc[pi][:, :], AF.Square, r=[b_pc[pi], b_const], w=[b_csq[cc]], bias=cbias)
                        P.mm(pst[0][:, :], ones_f, csb[:, cc, :], cc == 0, cc == 7, r=[b_csb[cc], b_const], w=[b_pst[0]])
                        P.mm(pst[1][:, :], ones_f, csq[:, cc, :], cc == 0, cc == 7, r=[b_csq[cc], b_const], w=[b_pst[1]])
                        if tt > 0:
                            ln_apply(tt - 1, cc)
                    P.ts('dve', mean_sb[:, :], pst[0][:, :], 1.0 / 1024, None, ALU.mult, r=[b_pst[0]], w=[b_stat])
                    P.tt('dve', msq[:, :], mean_sb[:, :], mean_sb[:, :], ALU.mult, r=[b_stat], w=[b_stat])
                    P.stt(var[:, :], pst[1][:, :], 1.0 / 1024, msq[:, :], ALU.mult, ALU.subtract, r=[b_pst[1], b_stat], w=[b_stat])
                    P.ts('dve', var[:, :], var[:, :], EPS, None, ALU.add, r=[b_stat], w=[b_stat])
                    P.act(msq[:, :], var[:, :], AF.Ln, r=[b_stat], w=[b_stat])
                    P.act(rstd[:, :], msq[:, :], AF.Exp, r=[b_stat], w=[b_stat], scale=-0.5)
                for cc in range(8):
                    ln_apply(NQ - 1, cc)
                P.emit("ph2")
            return b_out

        b_scr2 = phase2() if nph >= 2 else None

        def phase3():
            with ExitStack() as st:
                QA = [[sb(st, "QA%d_%d" % (c, i), [128, S], BF16) for i in range(2)] for c in range(2)]
                KA = [[sb(st, "KA%d_%d" % (c, i), [128, S], BF16) for i in range(2)] for c in range(2)]
                Vt = [sb(st, "Vt%d" % i, [128, NT, 128], BF16) for i in range(2)]
                NPB, NSB = 4, 3
                PT = [sb(st, "PT%d" % i, [128, 512], BF16) for i in range(NPB)]
                rr = [sb(st, "rr%d" % i, [128, 512], F32) for i in range(2)]
                tq_ = [sb(st, "tq%d" % i, [128, 512], F32) for i in range(2)]
                A = [sb(st, "A%d" % i, [128, 256], F32) for i in range(2)]
                Asq = [sb(st, "Asq%d" % i, [128, 256], F32) for i in range(2)]
                lnv = [sb(st, "lnv%d" % i, [128, 256], F32) for i in range(2)]
                ostg = [sb(st, "aostg%d" % i, [128, 256], BF16) for i in range(2)]
                sT = [ps(st, "sT%d" % i, [128, 512]) for i in range(NSB)]
                OO = [ps(st, "OO%d" % i, [128, 512]) for i in range(2)]
                LL = [ps(st, "LL%d" % i, [128, 512]) for i in range(2)]
                ssb = ps(st, "ssb", [128, 512])
                b_QA = [Buf("QA0"), Buf("QA1")]
                b_KA = [Buf("KA0"), Buf("KA1")]
                b_Vt = [Buf("Vt0"), Buf("Vt1")]
                b_PT = [Buf("PT%d" % i) for i in range(NPB)]
                b_sT = [Buf("sT%d" % i) for i in range(NSB)]
                b_OO = [Buf("OO0"), Buf("OO1")]
                b_LL = [Buf("LL0"), Buf("LL1")]
                b_ssb = Buf("ssb")
                b_rr = [Buf("rr0"), Buf("rr1")]
                b_tq = [Buf("tq0"), Buf("tq1")]
                b_A = [Buf("A0"), Buf("A1")]
                b_Asq = [Buf("Asq0"), Buf("Asq1")]
                b_lnv = [Buf("lnv0"), Buf("lnv1")]
                b_ostg = [Buf("aostg0"), Buf("aostg1")]
                b_out = Buf("scr3")
                for i in range(2):
                    P.add('pool', lambda e, i=i: e.memset(QA[1][i][0:64, :], 0.0), w=[b_QA[i]])
                    P.add('pool', lambda e, i=i: e.memset(QA[0][i][64:128, :], 0.0), w=[b_QA[i]])
                    P.add('pool', lambda e, i=i: e.memset(KA[1][i][0:64, :], 0.0), w=[b_KA[i]])
                    P.add('pool', lambda e, i=i: e.memset(KA[0][i][64:128, :], 0.0), w=[b_KA[i]])
                    P.add('pool', lambda e, i=i: e.memset(KA[0][i][64:66, :], 1.0), w=[b_KA[i]])
                    P.add('pool', lambda e, i=i: e.memset(KA[1][i][32:34, :], 1.0), w=[b_KA[i]])
                VVv = VV.ap().rearrange("(n p) d -> p n d", p=128)
                mh256 = sb(st, "mh256", [128, 256], F32)
                b_mh = Buf("mh256")
                P.add('pool', lambda e: e.memset(mh256[:, :], -0.5), w=[b_mh])

                def hload(h):
                    s_ = h % 2
                    P.dma('sp', QA[0][s_][0:64, :], QT[h, 0:64, :], r=[b_scr1], w=[b_QA[s_]], key=("QA", s_))
                    P.dma('sp', QA[1][s_][64:128, :], QT[h, 64:128, :], r=[b_scr1], ww=[b_QA[s_]], key=("QA", s_))
                    P.dma('sp', QA[0][s_][64:66, :], qaug_d[h, :, :], ww=[b_QA[s_]], key=("QA", s_))
                    P.dma('sp', QA[1][s_][32:34, :], qaug_d[h, :, :], ww=[b_QA[s_]], key=("QA", s_))
                    P.dma('sp', KA[0][s_][0:64, :], KT[h, 0:64, :], r=[b_scr1], w=[b_KA[s_]], key=("KA", s_))
                    P.dma('sp', KA[1][s_][64:128, :], KT[h, 64:128, :], r=[b_scr1], ww=[b_KA[s_]], key=("KA", s_))
                    P.dma('sp', Vt[s_][:, :, :], VVv[:, :, h * 128:(h + 1) * 128], r=[b_scr1], w=[b_Vt[s_]], key=("Vt", s_))

                NQ2 = S // 256
                blocks = []
                nqt = 0
                for h in range(H):
                    for qt in range(NQ2):
                        nkb = 2 * (qt + 1)
                        for kb in range(nkb):
                            j = kb - 2 * qt
                            blocks.append(dict(h=h, qt=qt, kb=kb, j=j, cs=(j * 128 if j >= 0 else 0), nkb=nkb,
                                               sb=len(blocks) % NSB, pb=len(blocks) % NPB, ob=nqt % 2, ep=nqt))
                        nqt += 1

                def qk(b):
                    s_, cs, q0 = b['h'] % 2, b['cs'], b['qt'] * 256
                    diag = b['j'] >= 0
                    for c in range(2):
                        P.mm(sT[b['sb']][:, c * 256 + cs:(c + 1) * 256], KA[c][s_][:, b['kb'] * 128:(b['kb'] + 1) * 128],
                             QA[c][s_][:, q0 + cs:q0 + 256], True, not diag, r=[b_KA[s_], b_QA[s_]],
                             w=[b_sT[b['sb']]] if c == 0 else [], ww=[] if c == 0 else [b_sT[b['sb']]])
                        if diag:
                            d0 = c * 256 + cs
                            P.mm(sT[b['sb']][:, d0:d0 + 128], ident_b, negtri_b, False, True, r=[b_const], ww=[b_sT[b['sb']]])

                def rest(b):
                    h, s_, cs, kb, nkb = b['h'], b['h'] % 2, b['cs'], b['kb'], b['nkb']
                    sbi, pb, ob = b['sb'], b['pb'], b['ob']
                    col = ALIB + h * ND + (2 * b['qt'] - kb + 3)
                    bias = cf[:, col:col + 1]
                    if cs == 0:
                        rngs = [(0, 512)]
                    else:
                        rngs = [(cs, 256), (256 + cs, 512)]
                    for ri, (a0, a1) in enumerate(rngs):
                        P.act(PT[pb][:, a0:a1], sT[sbi][:, a0:a1], AF.Exp, r=[b_sT[sbi], b_const],
                              w=[b_PT[pb]] if ri == 0 else [], ww=[] if ri == 0 else [b_PT[pb]], bias=bias, scale=0.125)
                    for ri, (a0, a1) in enumerate(rngs):
                        P.mm(OO[ob][:, a0:a1], Vt[s_][:, kb, :], PT[pb][:, a0:a1], kb == 0, kb == nkb - 1,
                             r=[b_Vt[s_], b_PT[pb]], w=[b_OO[ob]])
                        P.mm(LL[ob][:, a0:a1], ones_b, PT[pb][:, a0:a1], kb == 0, kb == nkb - 1,
                             r=[b_PT[pb], b_const], w=[b_LL[ob]])

                def epi1(b):
                    ob, e2 = b['ob'], b['ep'] % 2
                    P.add('dve', lambda e: e.reciprocal(rr[e2][:, :], LL[ob][:, :]), r=[b_LL[ob]], w=[b_rr[e2]])
                    P.tt('dve', tq_[e2][:, :], OO[ob][:, :], rr[e2][:, :], ALU.mult, r=[b_OO[ob], b_rr[e2]], w=[b_tq[e2]])
                    P.stt(A[e2][:, :], tq_[e2][:, 256:512], neglam, tq_[e2][:, 0:256], ALU.mult, ALU.add, r=[b_tq[e2], b_sm], w=[b_A[e2]])
                    P.tt('dve', Asq[e2][:, :], A[e2][:, :], A[e2][:, :], ALU.mult, r=[b_A[e2]], w=[b_Asq[e2]])

                def epi2a(b):
                    e2 = b['ep'] % 2
                    P.mm(ssb[:, 0:256], ones_f, Asq[e2][:, :], True, True, r=[b_Asq[e2], b_const], w=[b_ssb])
                    P.ts('dve', lnv[e2][:, :], ssb[:, 0:256], 1.0 / 128, EPS, ALU.mult, ALU.add, r=[b_ssb], w=[b_lnv[e2]])

                def epi2b(b):
                    e2, h, q0 = b['ep'] % 2, b['h'], b['qt'] * 256
                    P.act(Asq[e2][:, :], lnv[e2][:, :], AF.Ln, r=[b_lnv[e2]], w=[b_Asq[e2]])
                    P.act(lnv[e2][:, :], Asq[e2][:, :], AF.Exp, r=[b_Asq[e2]], w=[b_lnv[e2]], scale=-0.5)
                    P.stt(ostg[e2][:, :], A[e2][:, :], g08, lnv[e2][:, :], ALU.mult, ALU.mult, r=[b_A[e2], b_lnv[e2], b_sm], w=[b_ostg[e2]])
                    P.dma('sp', MIXT[h * 128:(h + 1) * 128, q0:q0 + 256], ostg[e2][:, :], r=[b_ostg[e2]], ww=[b_out], key=("aostg", e2))

                hload(0)
                if H > 1:
                    hload(1)
                wov = w_out.ap().rearrange("(k p) c -> p k c", p=128)
                for kq in range(4):
                    for ch in range(2):
                        P.dma('pool', wo[:, kq * 4:(kq + 1) * 4, ch * 1024:(ch + 1) * 1024],
                              wov[:, kq * 4:(kq + 1) * 4, ch * 1024:(ch + 1) * 1024], ww=[b_wo], key="wo")
                LOOK = NSB - 1
                pend = []
                nb = len(blocks)
                for i in range(min(LOOK, nb)):
                    qk(blocks[i])
                for i in range(nb):
                    if i + LOOK < nb:
                        qk(blocks[i + LOOK])
                    b = blocks[i]
                    rest(b)
                    for ent in pend:
                        ent[0] -= 1
                    for ent in [e_ for e_ in pend if e_[0] <= 0]:
                        pend.remove(ent)
                        if ent[1] == 'a':
                            epi2a(ent[2])
                            pend.append([3, 'b', ent[2]])
                        else:
                            epi2b(ent[2])
                    if b['kb'] == b['nkb'] - 1:
                        for ent in list(pend):
                            if ent[2]['ep'] <= b['ep'] - 2:
                                pend.remove(ent)
                                if ent[1] == 'a':
                                    epi2a(ent[2])
                                epi2b(ent[2])
                        epi1(b)
                        pend.append([12, 'a', b])
                    if b['kb'] == b['nkb'] - 1 and b['qt'] == NQ2 - 1 and b['h'] + 2 < H:
                        hload(b['h'] + 2)
                for ent in pend:
                    if ent[1] == 'a':
                        epi2a(ent[2])
                    epi2b(ent[2])
                P.emit("ph3")
            return b_out

        mid = ExitStack()
        top.enter_context(mid)
        wo = sb(mid, "wo", [128, KC, D], BF16)
        b_wo = Buf("wo")
        b_scr3 = phase3() if nph >= 3 else None

        def phase4():
            with ExitStack() as st:
                g2 = sb(st, "g2", [128, D], F32)
                brt = sb(st, "brt", [128, 36], F32)
                mT = [sb(st, "mT%d" % i, [128, KC, 512], BF16) for i in range(2)]
                xt = [sb(st, "x4_%d" % i, [128, D], F32) for i in range(2)]
                hs = [sb(st, "hs%d" % i, [128, D], F32) for i in range(2)]
                xn2 = sb(st, "xn2", [128, D], F32)
                xn2b = [sb(st, "xn2b%d" % i, [128, D], BF16) for i in range(2)]
                jk = sb(st, "jk4", [128, D], BF16)
                xT32 = sb(st, "xT32", [128, KC, 128], F32)
                rs = [sb(st, "rs%d" % i, [128, 160], F32) for i in range(2)]
                cum = [sb(st, "cum%d" % i, [128, 32], F32) for i in range(2)]
                po = [ps(st, "po%d" % i, [128, 512]) for i in range(4)]
                ptr = ps(st, "ptr", [128, 1024])
                prt = ps(st, "prt", [128, 512])
                ppre = ps(st, "ppre", [128, 512])
                b_g2 = Buf("g2")
                b_mT = [Buf("mT0"), Buf("mT1")]
                b_xt = [Buf("x40"), Buf("x41")]
                b_hs = [Buf("hs0"), Buf("hs1")]
                b_xn2, b_jk, b_xT32 = Buf("xn2"), Buf("jk4"), Buf("xT32")
                b_xn2b = [Buf("xn2b0"), Buf("xn2b1")]
                b_rs = [Buf("rs0"), Buf("rs1")]
                b_cum = [Buf("cum0"), Buf("cum1")]
                b_po = [Buf("po%d" % i) for i in range(4)]
                b_ptr, b_prt, b_ppre = Buf("ptr"), Buf("prt"), Buf("ppre")
                b_H, b_XS = Buf("HH"), Buf("XS")
                P.dma('sp', g2[:, :], bc_d[:, 0:D], w=[b_g2], key="g2")
                P.dma('sp', brt[:, :], bc_d[:, 2 * D:2 * D + 36], ww=[b_g2], key="g2")
                P.add('dve', lambda e: e.memset(cum[0][:, :], 0.0), w=[b_cum[0]])
                MIXv = MIXT.ap().rearrange("(k p) t -> p k t", p=128)

                def mload(tq):
                    P.dma('sp', mT[tq % 2][:, :, :], MIXv[:, :, tq * 512:(tq + 1) * 512], r=[b_scr2, b_scr3], w=[b_mT[tq % 2]], key=("mT", tq % 2))

                def xload(i):
                    P.dma('sp', xt[i % 2][:, :], x[i * 128:(i + 1) * 128, :], w=[b_xt[i % 2]], key=("x4", i % 2))

                def mm_tile(i):
                    tq, sub = i // 4, i % 4
                    m, bm = mT[tq % 2], b_mT[tq % 2]
                    for cbk in range(4):
                        for k in range(KC):
                            P.mm(po[cbk][:, :], m[:, k, sub * 128:(sub + 1) * 128], wo[:, k, cbk * 512:(cbk + 1) * 512], k == 0, k == KC - 1,
                                 r=[bm, b_wo], w=[b_po[cbk]])

                def adds(i):
                    h_, bh = hs[i % 2], b_hs[i % 2]
                    for cbk in range(4):
                        P.tt('dve', h_[:, cbk * 512:(cbk + 1) * 512], po[cbk][:, :], xt[i % 2][:, cbk * 512:(cbk + 1) * 512], ALU.add,
                             r=[b_po[cbk], b_xt[i % 2]], w=[bh] if cbk == 0 else [], ww=[] if cbk == 0 else [bh])
                    P.dma('sp', HH[i * 128:(i + 1) * 128, :], h_[:, :], r=[bh], ww=[b_H], key=("hs", i % 2))

                def part1(i):
                    h_, bh = hs[i % 2], b_hs[i % 2]
                    R, bR = rs[i % 2], b_rs[i % 2]
                    P.ttr(jk[:, :], h_[:, :], h_[:, :], R[:, 0:1], r=[bh], w=[b_jk, bR])
                    P.ts('dve', R[:, 1:2], R[:, 0:1], 1.0 / D, EPS, ALU.mult, ALU.add, r=[bR], w=[bR])
                    P.tt('pool', R[:, 2:3], R[:, 1:2], mhalf, ALU.pow, r=[bR, b_sm], w=[bR])
                    P.stt(xn2[:, :], h_[:, :], R[:, 2:3], g2[:, :], ALU.mult, ALU.mult, r=[bh, bR, b_g2], w=[b_xn2])
                    P.cp('act', xn2b[i % 2][:, :], xn2[:, :], r=[b_xn2], w=[b_xn2b[i % 2]])

                def part2(i):
                    R, bR = rs[i % 2], b_rs[i % 2]
                    for hb in range(2):
                        for j in range(8):
                            k = hb * 8 + j
                            P.tr(ptr[:, j * 128:(j + 1) * 128], xn2[:, k * 128:(k + 1) * 128], ident, r=[b_xn2, b_const], w=[b_ptr])
                        P.cp('act' if hb == 0 else 'dve', xT32[:, hb * 8:(hb + 1) * 8, :], ptr[:, :].rearrange("p (k t) -> p k t", t=128),
                             r=[b_ptr], w=[b_xT32] if hb == 0 else [], ww=[] if hb == 0 else [b_xT32])
                    for k in range(KC):
                        P.mm(prt[:, 0:36], xT32[:, k, :], wr[:, k * 36:(k + 1) * 36], k == 0, k == KC - 1, r=[b_xT32, b_const], w=[b_prt])
                    lg = R[:, 4:40]
                    P.tt('dve', lg, prt[:, 0:36], brt[:, :], ALU.add, r=[b_prt, b_g2], w=[bR])
                    P.add('dve', lambda e, R=R: e.reduce_max(R[:, 40:41], R[:, 4:8], AX.X), r=[bR], w=[bR])
                    P.ts('dve', R[:, 41:42], R[:, 40:41], -1.0, None, ALU.mult, r=[bR], w=[bR])
                    P.act(R[:, 44:48], R[:, 4:8], AF.Exp, r=[bR], w=[bR], bias=R[:, 41:42], accum_out=R[:, 42:43])
                    P.add('dve', lambda e, R=R: e.reciprocal(R[:, 43:44], R[:, 42:43]), r=[bR], w=[bR])
                    P.ts('dve', R[:, 48:52], R[:, 4:8], R[:, 40:41], None, ALU.is_equal, r=[bR], w=[bR])
                    goh_b = bass.AP(R, R[:, 48:52].offset, [list(R[:, 48:52].ap[0]), [1, 4], [0, 8]])
                    P.ts('dve', R[:, 52:84].rearrange("p (g e) -> p g e", e=8), goh_b, 1e30, -1e30, ALU.mult, ALU.add, r=[bR], w=[bR])
                    P.tt('dve', R[:, 84:116], R[:, 8:40], R[:, 52:84], ALU.add, r=[bR], w=[bR])
                    P.add('dve', lambda e, R=R: e.max(R[:, 116:124], R[:, 84:116]), r=[bR], w=[bR])
                    P.ts('dve', R[:, 52:84], R[:, 84:116], R[:, 116:117], None, ALU.is_equal, r=[bR], w=[bR])
                    P.ts('dve', R[:, 124:156], R[:, 84:116], R[:, 117:118], None, ALU.is_equal, r=[bR], w=[bR])
                    P.tt('dve', R[:, 156:157], R[:, 117:118], R[:, 116:117], ALU.subtract, r=[bR], w=[bR])
                    P.act(R[:, 157:158], R[:, 156:157], AF.Exp, r=[bR], w=[bR])
                    P.ts('dve', R[:, 158:159], R[:, 157:158], 1.0, None, ALU.add, r=[bR], w=[bR])
                    P.add('dve', lambda e, R=R: e.reciprocal(R[:, 159:160], R[:, 158:159]), r=[bR], w=[bR])
                    P.tt('dve', rt[:, i:i + 1], R[:, 159:160], R[:, 43:44], ALU.mult, r=[bR], w=[b_rt])
                    P.tt('dve', rt[:, NT + i:NT + i + 1], R[:, 157:158], rt[:, i:i + 1], ALU.mult, r=[bR, b_rt], w=[b_rt])
                    P.tt('dve', R[:, 84:116], R[:, 52:84], R[:, 124:156], ALU.add, r=[bR], w=[bR])

                def part3(i):
                    R, bR = rs[i % 2], b_rs[i % 2]
                    P.mm(ppre[:, 0:32], triU, R[:, 84:116], True, True, r=[bR, b_const], w=[b_ppre])
                    P.mm(ppre[:, 32:64], ones_f, R[:, 84:116], True, True, r=[bR, b_const], w=[b_ppre])
                    c0, c1 = cum[i % 2], cum[(i + 1) % 2]
                    P.tt('dve', R[:, 84:116], ppre[:, 0:32], c0[:, :], ALU.add, r=[b_ppre, b_cum[i % 2]], w=[bR])
                    P.tt('dve', c1[:, :], ppre[:, 32:64], c0[:, :], ALU.add, r=[b_ppre, b_cum[i % 2]], w=[b_cum[(i + 1) % 2]])
                    P.ts('dve', R[:, 84:116], R[:, 84:116], float(CAP - 1), None, ALU.min, r=[bR], w=[bR])
                    P.tt('dve', R[:, 84:116], R[:, 84:116], ebase, ALU.add, r=[bR, b_const], w=[bR])
                    P.stt(R[:, 4:36], R[:, 52:84], 1.0, R[:, 84:116], ALU.mult, ALU.mult, r=[bR], w=[bR], accum_out=R[:, 36:37])
                    P.stt(R[:, 4:36], R[:, 124:156], 1.0, R[:, 84:116], ALU.mult, ALU.mult, r=[bR], w=[bR], accum_out=R[:, 37:38])
                    P.cp('dve', rti[:, i:i + 1], R[:, 36:37], r=[bR], w=[b_rt])
                    P.cp('dve', rti[:, NT + i:NT + i + 1], R[:, 37:38], r=[bR], w=[b_rt])
                    P.cp('dve', rt[:, 2 * NT + i:2 * NT + i + 1], R[:, 36:37], r=[bR], w=[b_rt])
                    P.cp('dve', rt[:, 3 * NT + i:3 * NT + i + 1], R[:, 37:38], r=[bR], w=[b_rt])
                    for kk in range(2):
                        ia = rti[:, kk * NT + i:kk * NT + i + 1]
                        P.add('pool', lambda e, ia=ia, src=xn2b[i % 2]: e.indirect_dma_start(
                            out=XS[:, :], out_offset=bass.IndirectOffsetOnAxis(ap=ia, axis=0), in_=src[:, :], in_offset=None),
                            r=[b_xn2b[i % 2], b_rt], ww=[b_XS], dkey=("sc", i % 2, kk))

                mload(0)
                xload(0)
                if NQ > 1:
                    mload(1)
                mm_tile(0)
                adds(0)
                for i in range(NT):
                    tq, sub = i // 4, i % 4
                    if sub == 3 and tq + 2 < NQ:
                        mload(tq + 2)
                    if i + 1 < NT:
                        xload(i + 1)
                    part1(i)
                    if i + 1 < NT:
                        mm_tile(i + 1)
                        adds(i + 1)
                    if i >= 1:
                        part3(i - 1)
                    part2(i)
                part3(NT - 1)
                if dbg:
                    P.dma('sp', RT[:, :], rt[:, :], r=[b_rt], w=[Buf("RTd")], key="rtd")
                P.emit("ph4")
            return b_H, b_XS

        if nph >= 4:
            b_H, b_XS = phase4()
        mid.close()

        def phase5():
            with ExitStack() as st:
                NR = 32
                ring = [sb(st, "ring%d" % i, [128, 2048], BF16) for i in range(NR)]
                NXB = 2 * NST
                xsb = [sb(st, "xsb%d" % i, [128, D], BF16) for i in range(NXB)]
                xT = sb(st, "xT5", [128, KC, CAP], BF16)
                hT = sb(st, "hT5", [128, 8, CAP], BF16)
                sgt = [sb(st, "sg5_%d" % i, [128, CAP], F32) for i in range(2)]
                ystg = [sb(st, "ystg%d" % i, [128, D], F32) for i in range(2)]
                ptx = ps(st, "ptx", [128, 2048], BF16)
                pg = [ps(st, "pg%d" % i, [128, 512]) for i in range(2)]
                pu = [ps(st, "pu%d" % i, [128, 512]) for i in range(2)]
                pd = [ps(st, "pd%d" % i, [128, 512]) for i in range(2)]
                b_ring = [Buf("ring%d" % i) for i in range(NR)]
                b_xsb = [Buf("xsb%d" % i) for i in range(NXB)]
                b_xT = [Buf("xT5_%d" % i) for i in range(NST)]
                b_hT = [Buf("hT5_%d" % i) for i in range(8)]
                b_sgt = [Buf("sg50"), Buf("sg51")]
                b_ystg = [Buf("ystg0"), Buf("ystg1")]
                b_ptx = Buf("ptx")
                b_pg, b_pu, b_pd = [Buf("pg0"), Buf("pg1")], [Buf("pu0"), Buf("pu1")], [Buf("pd0"), Buf("pd1")]
                b_YS = Buf("YS")
                cnt = {'r': 0, 'x': 0, 'pd': 0, 'y': 0, 'ev': 0}

                def wloads(e):
                    sg, sd = [], []
                    for k in range(KC):
                        s_ = cnt['r'] % NR
                        cnt['r'] += 1
                        P.dma('pool', ring[s_][:, 0:1024], w_gate[e, k * 128:(k + 1) * 128, :], w=[b_ring[s_]], key=("ring", s_))
                        P.dma('pool', ring[s_][:, 1024:2048], w_up[e, k * 128:(k + 1) * 128, :], ww=[b_ring[s_]], key=("ring", s_))
                        sg.append(s_)
                    for k in range(8):
                        s_ = cnt['r'] % NR
                        cnt['r'] += 1
                        P.dma('pool', ring[s_][:, :], w_down[e, k * 128:(k + 1) * 128, :], w=[b_ring[s_]], key=("ring", s_))
                        sd.append(s_)
                    return sg, sd

                def xloads(e):
                    ids = []
                    for s3 in range(NST):
                        xi = cnt['x'] % NXB
                        cnt['x'] += 1
                        r0 = e * CAP + s3 * 128
                        P.dma('sp', xsb[xi][:, :], XS[r0:r0 + 128, :], r=[b_XS], w=[b_xsb[xi]], key=("xsb", xi))
                        ids.append(xi)
                    return ids

                nw = wloads(0)
                nx = xloads(0)
                for e in range(NE):
                    (sg, sd), xids = nw, nx
                    if e + 1 < NE:
                        nx = xloads(e + 1)
                    for s3 in range(NST):
                        xi = xids[s3]
                        for k in range(KC):
                            P.tr(ptx[:, k * 128:(k + 1) * 128], xsb[xi][:, k * 128:(k + 1) * 128], ident_b, r=[b_xsb[xi], b_const], w=[b_ptx])
                        for hb in range(2):
                            P.cp('act' if hb == 0 else 'dve', xT[:, hb * 8:(hb + 1) * 8, s3 * 128:(s3 + 1) * 128],
                                 ptx[:, hb * 1024:(hb + 1) * 1024].rearrange("p (k t) -> p k t", t=128),
                                 r=[b_ptx], w=[b_xT[s3]] if hb == 0 else [], ww=[] if hb == 0 else [b_xT[s3]])
                    for fe in range(8):
                        pi = fe % 2
                        for k in range(KC):
                            P.mm(pg[pi][:, 0:CAP], ring[sg[k]][:, fe * 128:(fe + 1) * 128], xT[:, k, :], k == 0, k == KC - 1,
                                 r=[b_ring[sg[k]]] + b_xT, w=[b_pg[pi]])
                        for k in range(KC):
                            P.mm(pu[pi][:, 0:CAP], ring[sg[k]][:, 1024 + fe * 128:1024 + (fe + 1) * 128], xT[:, k, :], k == 0, k == KC - 1,
                                 r=[b_ring[sg[k]]] + b_xT, w=[b_pu[pi]])
                        P.act(sgt[pi][:, :], pg[pi][:, 0:CAP], AF.Silu, r=[b_pg[pi]], w=[b_sgt[pi]])
                        P.tt('dve', hT[:, fe, :], sgt[pi][:, :], pu[pi][:, 0:CAP], ALU.mult, r=[b_sgt[pi], b_pu[pi]], w=[b_hT[fe]])
                    if e + 1 < NE:
                        nw = wloads(e + 1)
                    for s3 in range(NST):
                        yi = cnt['y'] % 2
                        cnt['y'] += 1
                        for cbk in range(4):
                            pi = cnt['pd'] % 2
                            cnt['pd'] += 1
                            for k in range(8):
                                P.mm(pd[pi][:, :], hT[:, k, s3 * 128:(s3 + 1) * 128], ring[sd[k]][:, cbk * 512:(cbk + 1) * 512], k == 0, k == 7,
                                     r=[b_hT[k], b_ring[sd[k]]], w=[b_pd[pi]])
                            cnt['ev'] += 1
                            P.cp('act' if cnt['ev'] % 2 else 'dve', ystg[yi][:, cbk * 512:(cbk + 1) * 512], pd[pi][:, :],
                                 r=[b_pd[pi]], w=[b_ystg[yi]] if cbk == 0 else [], ww=[] if cbk == 0 else [b_ystg[yi]])
                        r0 = e * CAP + s3 * 128
                        P.dma('sp', YS[r0:r0 + 128, :], ystg[yi][:, :], r=[b_ystg[yi]], ww=[b_YS], key=("ystg", yi))
                P.emit("ph5")
            return b_YS

        if nph >= 5:
            b_YS = phase5()

        def phase6():
            with ExitStack() as st:
                gf = sb(st, "gf", [128, D], F32)
                hb_ = [sb(st, "h6_%d" % i, [128, D], F32) for i in range(2)]
                y1 = [sb(st, "y1_%d" % i, [128, D], F32) for i in range(2)]
                y2 = [sb(st, "y2_%d" % i, [128, D], F32) for i in range(2)]
                ob = [sb(st, "ob%d" % i, [128, D], F32) for i in range(2)]
                jk = sb(st, "jk6", [128, D], BF16)
                s6 = [sb(st, "s6_%d" % i, [128, 4], F32) for i in range(2)]
                b_gf = Buf("gf")
                b_h = [Buf("h60"), Buf("h61")]
                b_y1 = [Buf("y10"), Buf("y11")]
                b_y2 = [Buf("y20"), Buf("y21")]
                b_ob = [Buf("ob0"), Buf("ob1")]
                b_jk = Buf("jk6")
                b_s6 = [Buf("s60"), Buf("s61")]
                b_fin = Buf("fin")
                P.dma('sp', gf[:, :], bc_d[:, D:2 * D], w=[b_gf], key="gf")

                def loads(i):
                    a = i % 2
                    P.dma('sp', hb_[a][:, :], HH[i * 128:(i + 1) * 128, :], r=[b_H], w=[b_h[a]], key=("h6", a))
                    for kk, (yy, by) in enumerate(((y1, b_y1), (y2, b_y2))):
                        ia = rti[:, kk * NT + i:kk * NT + i + 1]
                        P.add('pool', lambda e, ia=ia, dst=yy[a]: e.indirect_dma_start(
                            out=dst[:, :], out_offset=None, in_=YS[:, :], in_offset=bass.IndirectOffsetOnAxis(ap=ia, axis=0)),
                            r=[b_YS, b_rt], w=[by[a]], dkey=("ga", a, kk))
                loads(0)
                for i in range(NT):
                    if i + 1 < NT:
                        loads(i + 1)
                    a = i % 2
                    P.stt(hb_[a][:, :], y1[a][:, :], rt[:, i:i + 1], hb_[a][:, :], ALU.mult, ALU.add, r=[b_y1[a], b_rt], w=[b_h[a]])
                    P.stt(hb_[a][:, :], y2[a][:, :], rt[:, NT + i:NT + i + 1], hb_[a][:, :], ALU.mult, ALU.add, r=[b_y2[a], b_rt], w=[b_h[a]])
                    P.act(jk[:, :], hb_[a][:, :], AF.Square, r=[b_h[a]], w=[b_jk, b_s6[a]], accum_out=s6[a][:, 0:1])
                    P.ts('dve', s6[a][:, 1:2], s6[a][:, 0:1], 1.0 / D, EPS, ALU.mult, ALU.add, r=[b_s6[a]], w=[b_s6[a]])
                    P.tt('pool', s6[a][:, 2:3], s6[a][:, 1:2], mhalf, ALU.pow, r=[b_s6[a], b_sm], w=[b_s6[a]])
                    P.stt(ob[a][:, :], hb_[a][:, :], s6[a][:, 2:3], gf[:, :], ALU.mult, ALU.mult, r=[b_h[a], b_s6[a], b_gf], w=[b_ob[a]])
                    P.dma('sp', out_d[i * 128:(i + 1) * 128, :], ob[a][:, :], r=[b_ob[a]], ww=[b_fin], key=("ob", a))
                P.add('sp', lambda e: e.nop(), r=[b_fin])
                P.emit("ph6")

        if nph >= 6:
            phase6()
    return nc


def make_consts(S, CAP):
    NT = S // 128
    ND = NT + 3
    k = np.arange(128)
    cf = np.zeros((128, 416 + 8 * ND), np.float32)
    cf[:, 0:128] = np.eye(128, dtype=np.float32)
    cf[:, 128:256] = (k[:, None] < k[None, :]).astype(np.float32)
    cf[:, 256:384] = 1.0
    cf[:, 384:416] = (np.arange(32) * CAP)[None, :]
    sl = _slopes()
    for h in range(H):
        for di in range(ND):
            cf[:, 416 + h * ND + di] = sl[h] * (k - 128.0 * (di - 3))
    cb = np.zeros((128, 512), np.float32)
    cb[:, 384:512] = np.where(k[:, None] > k[None, :], -30000.0, 0.0)
    cb[:, 0:128] = np.eye(128)
    cb[:, 128:256] = 1.0
    cb[:, 256:384] = (k[:, None] <= k[None, :])
    cb = cb.astype(ml_dtypes.bfloat16)
    qi = np.arange(S) % 256
    lo = qi % 256
    qaug = np.stack([np.stack([-8.0 * sl[h] * lo, -8.0 * sl[h] * (qi - lo)]) for h in range(H)]).astype(np.float32).astype(ml_dtypes.bfloat16)
    return cf, cb, qaug


def make_params(inp):
    f = lambda a: np.asarray(a, np.float32)
    pp = np.zeros((128, 16 + 8 * TAPS + 24 + 1 + 256), np.float32)
    pp[:, 0:16] = f(inp['norm_mix_g'])[0].reshape(16, 128).T
    cw = f(inp['conv_w'])[0, :, 0, :]
    pp[:, 16:16 + 8 * TAPS] = cw.reshape(TAPS, 8, 128).transpose(2, 1, 0).reshape(128, 8 * TAPS)
    o = 16 + 8 * TAPS
    pp[:, o:o + 8] = f(inp['conv_b'])[0].reshape(8, 128).T
    pp[:, o + 8:o + 16] = f(inp['conv_ln_g'])[0].reshape(8, 128).T
    pp[:, o + 16:o + 24] = f(inp['conv_ln_b'])[0].reshape(8, 128).T
    pp[:, o + 24] = f(inp['subln_g'])[0]
    lam = np.concatenate([f(inp['lambda_q1'])[0], f(inp['lambda_k1'])[0], f(inp['lambda_q2'])[0], f(inp['lambda_k2'])[0]])
    pp[:, o + 25:o + 25 + 256] = lam[None, :]
    bc = np.zeros((128, 2 * D + 36), np.float32)
    bc[:, 0:D] = f(inp['norm_ffn_g'])[0][None, :]
    bc[:, D:2 * D] = f(inp['norm_final_g'])[None, :]
    bc[:, 2 * D:2 * D + 4] = f(inp['b_group_router'])[0][None, :]
    bc[:, 2 * D + 4:] = f(inp['b_expert_router'])[0][None, :]
    wrc = np.concatenate([f(inp['w_group_router'])[0], f(inp['w_expert_router'])[0]], axis=1)
    wr = np.ascontiguousarray(wrc.reshape(16, 128, 36).transpose(1, 0, 2).reshape(128, 16 * 36))
    return pp, bc, wr


def make_in_maps(inp, S, CAP, cores):
    cf, cb, qaug = make_consts(S, CAP)
    pp, bc, wr = make_params(inp)
    f = lambda a: np.ascontiguousarray(np.asarray(a, np.float32))
    shared = dict(w_in=f(inp['w_in'])[0], w_out=f(inp['w_out'])[0], w_gate=f(inp['w_gate'])[0], w_up=f(inp['w_up'])[0],
                  w_down=f(inp['w_down'])[0], cf=cf, cb=cb, qaug=qaug, pp=pp, bc=bc, wr=wr)
    xs = np.asarray(inp['x'], np.float32)
    maps = []
    for b in cores:
        m = dict(shared)
        m['x'] = np.ascontiguousarray(xs[b, :S])
        maps.append(m)
    return maps


_NC_CACHE = {}


def kernel(**inputs):
    S, CAP = 4096, 384
    if 'nc' not in _NC_CACHE:
        _NC_CACHE['nc'] = build(S, CAP)
    nc = _NC_CACHE['nc']
    maps = make_in_maps(inputs, S, CAP, list(range(8)))
    res = run_bass_kernel_spmd(nc, maps, core_ids=list(range(8)))
    return np.stack([np.asarray(r["out"], np.float32) for r in res.results], axis=0)
```

```python
import math
from contextlib import ExitStack
import numpy as np
import ml_dtypes
import concourse.bass as bass
import concourse.mybir as mybir
from concourse.bass_utils import run_bass_kernel_spmd

F32 = mybir.dt.float32
BF16 = mybir.dt.bfloat16
I32 = mybir.dt.int32
AF = mybir.ActivationFunctionType
ALU = mybir.AluOpType
AX = mybir.AxisListType

D = 2048
KC = 16
H = 8
NE = 32
EPS = 1e-6
TAPS = 31
ENG = ['pe', 'act', 'dve', 'pool', 'sp']
SAME_ENGINE_SYNC = True


class Buf:
    __slots__ = ('name', 'wc', 'wd', 'rc', 'rd')

    def __init__(self, name):
        self.name = name
        self.wc = {}
        self.wd = {}
        self.rc = {}
        self.rd = {}


class Op:
    __slots__ = ('eng', 'fn', 'cdeps', 'ddeps', 'sig', 'sigval', 'dma', 'emitted')


class Prog:
    def __init__(self, nc, stack):
        self.nc = nc
        self.stack = stack
        self.ops = {e: [] for e in ENG}
        self.csem = {e: stack.enter_context(nc.semaphore("cs_" + e)) for e in ENG}
        self.ccount = {e: 0 for e in ENG}
        self.waited = {e: {} for e in ENG}
        self.dsems = {}
        self.nsem = len(ENG)

    def dsem(self, key):
        ent = self.dsems.get(key)
        if ent is None:
            ent = [self.stack.enter_context(self.nc.semaphore("ds%d" % len(self.dsems))), 0]
            self.dsems[key] = ent
            self.nsem += 1
        return ent

    def add(self, eng, fn, r=(), w=(), dkey=None, ww=()):
        op = Op()
        op.eng, op.fn, op.cdeps, op.ddeps = eng, fn, [], []
        op.sig, op.sigval, op.dma, op.emitted = False, None, None, False
        if dkey is not None:
            ent = self.dsem(dkey)
            ent[1] += 16
            op.dma = (ent[0], ent[1])

        def cdep(d):
            if d is op:
                return
            if d.eng != eng or op.dma is not None or (SAME_ENGINE_SYNC and eng != 'pe'):
                op.cdeps.append(d)
                if not d.emitted:
                    d.sig = True

        for b in r:
            for d in b.wc.values():
                cdep(d)
            for sv in b.wd.items():
                op.ddeps.append(sv)
        for b in w:
            for d in b.wc.values():
                cdep(d)
            for d in b.rc.values():
                cdep(d)
            for sv in b.wd.items():
                op.ddeps.append(sv)
            for sv in b.rd.items():
                op.ddeps.append(sv)
        for b in r:
            if b in w:
                continue
            if op.dma is not None:
                b.rd[op.dma[0]] = op.dma[1]
            else:
                b.rc[eng] = op
        for b in w:
            b.rc.clear()
            b.rd.clear()
            b.wc.clear()
            b.wd.clear()
            if op.dma is not None:
                b.wd[op.dma[0]] = op.dma[1]
            else:
                b.wc[eng] = op
        for b in ww:
            if op.dma is not None:
                b.wd[op.dma[0]] = op.dma[1]
            else:
                b.wc[eng] = op
        self.ops[eng].append(op)
        return op

    def emit(self, name):
        nc = self.nc
        for e in ENG:
            for op in self.ops[e]:
                if op.sig and op.dma is None:
                    self.ccount[e] += 1
                    op.sigval = self.ccount[e]
        endval = {}
        for e in ENG:
            self.ccount[e] += 1
            endval[e] = self.ccount[e]
        ops, csem, waited = self.ops, self.csem, self.waited

        def run(e, eng):
            wt = waited[e]
            for op in ops[e]:
                for d in op.cdeps:
                    if d.sigval is None:
                        continue
                    s = csem[d.eng]
                    if wt.get(s, 0) < d.sigval:
                        eng.wait_ge(s, d.sigval)
                        wt[s] = d.sigval
                for (s, v) in op.ddeps:
                    if wt.get(s, 0) < v:
                        eng.wait_ge(s, v)
                        wt[s] = v
                ins = op.fn(eng)
                if op.dma is not None:
                    ins.then_inc(op.dma[0], 16)
                elif op.sig:
                    ins.then_inc(csem[e], 1)
                op.emitted = True
            eng.drain().then_inc(csem[e], 1)
            for e2 in ENG:
                if e2 != e:
                    eng.wait_ge(csem[e2], endval[e2])
                    wt[csem[e2]] = endval[e2]

        with nc.Block(name) as block:
            @block.tensor
            def _(eng):
                run('pe', eng)

            @block.scalar
            def _(eng):
                run('act', eng)

            @block.vector
            def _(eng):
                run('dve', eng)

            @block.gpsimd
            def _(eng):
                run('pool', eng)

            @block.sync
            def _(eng):
                run('sp', eng)
        self.ops = {e: [] for e in ENG}

    def mm(self, out, lhsT, rhs, start, stop, r=(), w=(), ww=()):
        return self.add('pe', lambda e: e.matmul(out, lhsT, rhs, start=start, stop=stop), r, w, ww=ww)

    def tr(self, out, in_, ident, r=(), w=()):
        return self.add('pe', lambda e: e.transpose(out, in_, ident), r, w)

    def act(self, out, in_, func, r=(), w=(), ww=(), **kw):
        return self.add('act', lambda e: e.activation(out, in_, func, **kw), r, w, ww=ww)

    def ts(self, eng, out, in0, s1, s2, op0, op1=None, r=(), w=(), ww=(), **kw):
        if op1 is None:
            return self.add(eng, lambda e: e.tensor_scalar(out, in0, s1, None, op0, **kw), r, w, ww=ww)
        return self.add(eng, lambda e: e.tensor_scalar(out, in0, s1, s2, op0, op1, **kw), r, w, ww=ww)

    def tt(self, eng, out, in0, in1, op, r=(), w=(), ww=()):
        return self.add(eng, lambda e: e.tensor_tensor(out, in0, in1, op), r, w, ww=ww)

    def stt(self, out, in0, scalar, in1, op0, op1, r=(), w=(), **kw):
        return self.add('dve', lambda e: e.scalar_tensor_tensor(out, in0, scalar, in1, op0, op1, **kw), r, w)

    def ttr(self, out, in0, in1, accum, r=(), w=()):
        return self.add('dve', lambda e: e.scalar_tensor_tensor(out, in0, 1.0, in1, ALU.mult, ALU.mult, accum_out=accum), r, w)

    def cp(self, eng, out, in_, r=(), w=(), ww=()):
        if eng == 'act':
            return self.add('act', lambda e: e.copy(out, in_), r, w, ww=ww)
        return self.add(eng, lambda e: e.tensor_copy(out, in_), r, w, ww=ww)

    def dma(self, q, out, in_, r=(), w=(), key=None, ww=(), **kw):
        return self.add(q, lambda e: e.dma_start(out, in_, **kw), r, w, dkey=key, ww=ww)


def _slopes():
    return [2.0 ** (-8.0 * (i + 1) / H) for i in range(H)]


def build(S=4096, CAP=384, dbg=False, nph=99):
    NT = S // 128
    NQ = S // 512
    NHALF = max(1, S // 2048)
    HS = S // NHALF
    ND = NT + 3
    NST = CAP // 128
    nc = bass.Bass("TRN2", target_bir_lowering=False)

    def din(name, shape, dt=F32):
        return nc.dram_tensor(name, list(shape), dt, kind="ExternalInput")

    def dscr(name, shape, dt):
        return nc.dram_tensor(name, list(shape), dt, kind="ExternalOutput" if dbg else "Internal")

    x = din("x", [S, D])
    w_in = din("w_in", [D, 5120])
    w_out = din("w_out", [D, D])
    w_gate = din("w_gate", [NE, D, 1024])
    w_up = din("w_up", [NE, D, 1024])
    w_down = din("w_down", [NE, 1024, D])
    cf_d = din("cf", [128, 416 + 8 * ND])
    cb_d = din("cb", [128, 512], BF16)
    qaug_d = din("qaug", [H, 2, S], BF16)
    pp_d = din("pp", [128, 16 + 8 * TAPS + 24 + 1 + 256])
    bc_d = din("bc", [128, 2 * D + 36])
    wr_d = din("wr", [128, KC * 36])
    out_d = nc.dram_tensor("out", [S, D], F32, kind="ExternalOutput")

    QT = dscr("QT", [H, 128, S], BF16)
    KT = dscr("KT", [H, 128, S], BF16)
    VV = dscr("VV", [S, 1024], BF16)
    UT = dscr("UT", [1024, S], BF16)
    MIXT = dscr("MIXT", [D, S], BF16)
    HH = dscr("HH", [S, D], F32)
    XS = dscr("XS", [NE * CAP, D], BF16)
    YS = dscr("YS", [NE * CAP, D], F32)
    RT = dscr("RT", [128, 4 * NT], F32)

    top = ExitStack()
    with top:
        P = Prog(nc, top)

        def sb(stack, name, shape, dt):
            return stack.enter_context(nc.sbuf_tensor("s_" + name, list(shape), dt))

        def ps(stack, name, shape, dt=F32):
            return stack.enter_context(nc.psum_tensor("p_" + name, list(shape), dt))

        cf = sb(top, "cf", [128, 416 + 8 * ND], F32)
        cb = sb(top, "cb", [128, 512], BF16)
        pp = sb(top, "pp", [128, 16 + 8 * TAPS + 24 + 1 + 256], F32)
        wr = sb(top, "wr", [128, KC * 36], F32)
        rt = sb(top, "rt", [128, 4 * NT], F32)
        rti = sb(top, "rti", [128, 2 * NT], I32)
        sm = sb(top, "sm", [128, 16], F32)
        b_const = Buf("const")
        b_rt = Buf("rt")
        ident = cf[:, 0:128]
        triU = cf[:, 128:256]
        ones_f = cf[:, 256:384]
        ebase = cf[:, 384:416]
        ALIB = 416
        ident_b = cb[:, 0:128]
        ones_b = cb[:, 128:256]
        tri_b = cb[:, 256:384]
        negtri_b = cb[:, 384:512]
        PP_G = 0
        PP_CW = 16
        PP_CB = PP_CW + 8 * TAPS
        PP_LG = PP_CB + 8
        PP_LB = PP_LG + 8
        PP_SG = PP_LB + 8
        PP_LAM = PP_SG + 1
        neglam = sm[:, 0:1]
        g08 = sm[:, 1:2]
        mhalf = sm[:, 2:3]

        b_sm = Buf("sm")
        P.dma('sp', cf[:, :], cf_d[:, :], w=[b_const], key="c0")
        P.dma('sp', cb[:, :], cb_d[:, :], w=[b_const], key="c0")
        P.dma('sp', pp[:, :], pp_d[:, :], w=[b_const], key="c0")
        P.dma('sp', wr[:, :], wr_d[:, :], w=[b_const], key="c0")
        lq = PP_LAM
        P.add('dve', lambda e: e.memset(sm[:, :], 0.0), w=[b_sm])
        P.add('dve', lambda e: e.memset(mhalf, -0.5), w=[b_sm])
        with ExitStack() as st0:
            junk = sb(st0, "junk0", [128, 64], F32)
            b_j = Buf("junk0")
            P.ttr(junk[:, :], pp[:, lq:lq + 64], pp[:, lq + 64:lq + 128], sm[:, 4:5], r=[b_const], w=[b_sm, b_j])
            P.ttr(junk[:, :], pp[:, lq + 128:lq + 192], pp[:, lq + 192:lq + 256], sm[:, 5:6], r=[b_const], w=[b_sm, b_j])
            P.act(sm[:, 6:8], sm[:, 4:6], AF.Exp, r=[b_sm], w=[b_sm])
            P.tt('dve', sm[:, 3:4], sm[:, 7:8], sm[:, 6:7], ALU.subtract, r=[b_sm], w=[b_sm])
            P.ts('dve', neglam, sm[:, 3:4], -(0.8 - 0.6), None, ALU.add, r=[b_sm], w=[b_sm])
            P.ts('dve', g08, pp[:, PP_SG:PP_SG + 1], 1.0 - (0.8 - 0.6), None, ALU.mult, r=[b_sm, b_const], w=[b_sm])
            P.emit("ph0")

        def phase1():
            with ExitStack() as st:
                CS = min(1024, S)
                NCH = S // CS
                ntl = CS // 128
                xnT2 = [sb(st, "xnT%d" % i, [128, KC, CS], BF16) for i in range(2)]
                NW = 4
                Wb = [sb(st, "Wb%d" % i, [128, KC, 512], BF16) for i in range(NW)]
                stg = [sb(st, "stg%d" % i, [128, CS], BF16) for i in range(3)]
                xt = [sb(st, "xt%d" % i, [128, D], F32) for i in range(2)]
                jk = sb(st, "jk", [128, D], BF16)
                sgt = [sb(st, "sgt%d" % i, [128, 512], F32) for i in range(2)]
                st1 = sb(st, "st1", [128, 8], F32)
                tp = [ps(st, "tp%d" % i, [128, 512]) for i in range(2)]
                pj = [ps(st, "pj%d" % i, [128, 512]) for i in range(4)]
                b_xnT2 = [[Buf("xnT%d_%d" % (u, i)) for i in range(ntl)] for u in range(2)]
                b_W = [Buf("W%d" % i) for i in range(NW)]
                b_stg = [Buf("stg%d" % i) for i in range(3)]
                b_xt = [Buf("xt%d" % i) for i in range(2)]
                b_jk = Buf("jk")
                b_sgt = [Buf("sgt%d" % i) for i in range(2)]
                b_st1 = [Buf("st1a"), Buf("st1b")]
                b_tp = [Buf("tp0"), Buf("tp1")]
                b_pj = [Buf("pj%d" % i) for i in range(4)]
                b_scr = Buf("scr1")
                cnt = {'w': 0, 'stg': 0, 'pj': 0, 'ev': 0}

                def evac(out, in_, r, w):
                    cnt['ev'] += 1
                    if cnt['ev'] % 2:
                        P.cp('act', out, in_, r=r, w=w)
                    else:
                        P.cp('dve', out, in_, r=r, w=w)

                def wload(c0):
                    s = cnt['w'] % NW
                    cnt['w'] += 1
                    src = w_in[:, c0:c0 + 512].rearrange("(k p) c -> p k c", p=128)
                    for kq in range(4):
                        P.dma('pool', Wb[s][:, kq * 4:(kq + 1) * 4, :], src[:, kq * 4:(kq + 1) * 4, :], w=[b_W[s]], key=("W", s))
                    return s

                def xload(gt):
                    P.dma('sp', xt[gt % 2][:, :], x[gt * 128:(gt + 1) * 128, :], w=[b_xt[gt % 2]], key=("xt", gt % 2))

                def norm_pre(gt):
                    xs_, bx = xt[gt % 2], b_xt[gt % 2]
                    a = (gt % 2) * 4
                    bs = b_st1[gt % 2]
                    P.ttr(jk[:, :], xs_[:, :], xs_[:, :], st1[:, a:a + 1], r=[bx], w=[b_jk, bs])
                    P.ts('dve', st1[:, a + 1:a + 2], st1[:, a:a + 1], 1.0 / D, EPS, ALU.mult, ALU.add, r=[bs], w=[bs])
                    P.tt('pool', st1[:, a + 2:a + 3], st1[:, a + 1:a + 2], mhalf, ALU.pow, r=[bs, b_sm], w=[bs])
                    P.ts('dve', xs_[:, :], xs_[:, :], st1[:, a + 2:a + 3], None, ALU.mult, r=[bs, bx], w=[bx])

                def norm_tr(gt):
                    c, i = gt // ntl, gt % ntl
                    xnT, b_xnT = xnT2[c % 2], b_xnT2[c % 2]
                    xs_, bx = xt[gt % 2], b_xt[gt % 2]
                    for g4 in range(4):
                        tpi, btp = tp[g4 % 2], b_tp[g4 % 2]
                        for j in range(4):
                            k = g4 * 4 + j
                            P.tr(tpi[:, j * 128:(j + 1) * 128], xs_[:, k * 128:(k + 1) * 128], ident, r=[bx, b_const], w=[btp])
                        for j in range(4):
                            k = g4 * 4 + j
                            o = xnT[:, k, i * 128:(i + 1) * 128]
                            gcol = pp[:, PP_G + k:PP_G + k + 1]
                            if j % 2 == 0:
                                P.act(o, tpi[:, j * 128:(j + 1) * 128], AF.Copy, r=[btp, b_const], w=[b_xnT[i]], scale=gcol)
                            else:
                                P.ts('dve', o, tpi[:, j * 128:(j + 1) * 128], gcol, None, ALU.mult, r=[btp, b_const], w=[b_xnT[i]])

                def step(k):
                    norm_tr(k)
                    if k + 2 < NT:
                        xload(k + 2)
                    if k + 1 < NT:
                        norm_pre(k + 1)

                worder = []
                for c_ in range(NCH):
                    worder += [0, 512, 1024, 1536, 2048, 2560, 3072, 4096, 3584, 4608]
                wstate = {'issued': 0}

                def need(idx):
                    while wstate['issued'] < min(len(worder), idx + 3):
                        wload(worder[wstate['issued']])
                        wstate['issued'] += 1
                    return idx % NW

                need(0)
                xload(0)
                if NT > 1:
                    xload(1)
                norm_pre(0)
                for gt in range(ntl):
                    step(gt)
                for c in range(NCH):
                    t0 = c * CS
                    xnT, b_xnT = xnT2[c % 2], b_xnT2[c % 2]
                    nxt_tiles = list(range((c + 1) * ntl, (c + 2) * ntl)) if c + 1 < NCH else []

                    def interleave():
                        if nxt_tiles:
                            step(nxt_tiles.pop(0))

                    def feat_major(ws, sub, dst_ap):
                        si = cnt['stg'] % 3
                        cnt['stg'] += 1
                        for tq in range(CS // 512):
                            pi = cnt['pj'] % 4
                            cnt['pj'] += 1
                            for k in range(KC):
                                P.mm(pj[pi][:, :], Wb[ws][:, k, sub * 128:(sub + 1) * 128], xnT[:, k, tq * 512:(tq + 1) * 512],
                                     k == 0, k == KC - 1, r=[b_W[ws]] + b_xnT[tq * 4:tq * 4 + 4], w=[b_pj[pi]])
                            evac(stg[si][:, tq * 512:(tq + 1) * 512], pj[pi][:, :], r=[b_pj[pi]], w=[b_stg[si]])
                        P.dma('sp', dst_ap, stg[si][:, :], r=[b_stg[si]], w=[b_scr], key=("stg", si))

                    wb0 = c * 10
                    for g in range(4):
                        ws = need(wb0 + g)
                        for sub in range(4):
                            hh = (g % 2) * 4 + sub
                            dst = (QT if g < 2 else KT)[hh, :, t0:t0 + CS]
                            feat_major(ws, sub, dst)
                        interleave()
                    for g in range(2):
                        ws = need(wb0 + 4 + g)
                        for i in range(ntl):
                            pi = cnt['pj'] % 4
                            cnt['pj'] += 1
                            for k in range(KC):
                                P.mm(pj[pi][:, :], xnT[:, k, i * 128:(i + 1) * 128], Wb[ws][:, k, :], k == 0, k == KC - 1,
                                     r=[b_W[ws], b_xnT[i]], w=[b_pj[pi]])
                            si = cnt['stg'] % 3
                            cnt['stg'] += 1
                            evac(stg[si][:, 0:512], pj[pi][:, :], r=[b_pj[pi]], w=[b_stg[si]])
                            P.dma('sp', VV[t0 + i * 128:t0 + (i + 1) * 128, g * 512:(g + 1) * 512], stg[si][:, 0:512],
                                  r=[b_stg[si]], w=[b_scr], key=("stg", si))
                        interleave()
                    for g in range(2):
                        wa = need(wb0 + 6 + 2 * g)
                        wg = need(wb0 + 7 + 2 * g)
                        for sub in range(4):
                            si = cnt['stg'] % 3
                            cnt['stg'] += 1
                            for tq in range(CS // 512):
                                pa = cnt['pj'] % 4
                                pg = (cnt['pj'] + 1) % 4
                                cnt['pj'] += 2
                                rr_ = b_xnT[tq * 4:tq * 4 + 4]
                                for k in range(KC):
                                    P.mm(pj[pa][:, :], Wb[wa][:, k, sub * 128:(sub + 1) * 128], xnT[:, k, tq * 512:(tq + 1) * 512],
                                         k == 0, k == KC - 1, r=[b_W[wa]] + rr_, w=[b_pj[pa]])
                                for k in range(KC):
                                    P.mm(pj[pg][:, :], Wb[wg][:, k, sub * 128:(sub + 1) * 128], xnT[:, k, tq * 512:(tq + 1) * 512],
                                         k == 0, k == KC - 1, r=[b_W[wg]] + rr_, w=[b_pj[pg]])
                                sgi = tq % 2
                                P.act(sgt[sgi][:, :], pj[pg][:, :], AF.Sigmoid, r=[b_pj[pg]], w=[b_sgt[sgi]])
                                P.tt('dve', stg[si][:, tq * 512:(tq + 1) * 512], pj[pa][:, :], sgt[sgi][:, :], ALU.mult,
                                     r=[b_pj[pa], b_sgt[sgi]], w=[b_stg[si]])
                            ch0 = g * 512 + sub * 128
                            P.dma('sp', UT[ch0:ch0 + 128, t0:t0 + CS], stg[si][:, :], r=[b_stg[si]], w=[b_scr], key=("stg", si))
                            if sub % 2 == 1:
                                interleave()
                    while nxt_tiles:
                        interleave()
                P.emit("ph1")
            return b_scr

        b_scr1 = phase1()

        def phase2():
            with ExitStack() as st:
                Dg = sb(st, "Dg", [128, 8 * TAPS, 128], BF16)
                U = [sb(st, "U%d" % i, [128, 8, 544], BF16) for i in range(2)]
                csb2 = [sb(st, "csb%d" % i, [128, 8, 512], F32) for i in range(2)]
                csq2 = [sb(st, "csq%d" % i, [128, 8, 512], F32) for i in range(2)]
                mean_sb = sb(st, "mean_sb", [128, 512], F32)
                msq = sb(st, "msq", [128, 512], F32)
                var = sb(st, "var", [128, 512], F32)
                rstd = sb(st, "rstd", [128, 512], F32)
                tmp = [sb(st, "tmp%d" % i, [128, 512], F32) for i in range(2)]
                tm2 = [sb(st, "tm2%d" % i, [128, 512], F32) for i in range(2)]
                ostg = [sb(st, "ostg%d" % i, [128, 512], BF16) for i in range(2)]
                pc = [ps(st, "pc%d" % i, [128, 512]) for i in range(2)]
                pst = [ps(st, "pst%d" % i, [128, 512]) for i in range(2)]
                b_Dg = [Buf("Dg%d" % i) for i in range(8)]
                b_U = [Buf("U0"), Buf("U1")]
                b_csb2 = [[Buf("csb%d_%d" % (u, i)) for i in range(8)] for u in range(2)]
                b_csq2 = [[Buf("csq%d_%d" % (u, i)) for i in range(8)] for u in range(2)]
                b_pc = [Buf("pc0"), Buf("pc1")]
                b_pst = [Buf("pst0"), Buf("pst1")]
                b_stat = Buf("stat")
                b_tmp = [Buf("tmp0"), Buf("tmp1")]
                b_tm2 = [Buf("tm20"), Buf("tm21")]
                b_ostg = [Buf("ostg0"), Buf("ostg1")]
                b_out = Buf("scr2")
                for cc in range(8):
                    for j in range(TAPS):
                        col = PP_CW + cc * TAPS + j
                        eng = 'dve'
                        P.ts(eng, Dg[:, cc * TAPS + j, :], ident, pp[:, col:col + 1], None, ALU.mult,
                             r=[b_const], ww=[b_Dg[cc]])
                UTv = UT.ap().rearrange("(c p) t -> p c t", p=128)

                def uload(tt):
                    u = tt % 2
                    if tt == 0:
                        P.add('pool', lambda e: e.memset(U[0][:, :, 0:30], 0.0), w=[b_U[0]])
                        P.dma('sp', U[0][:, :, 30:542], UTv[:, :, 0:512], r=[b_scr1], w=[b_U[0]], key=("U", 0))
                    else:
                        P.dma('sp', U[u][:, :, 0:542], UTv[:, :, tt * 512 - 30:tt * 512 + 512], r=[b_scr1], w=[b_U[u]], key=("U", u))
                uload(0)
                nn = {'n': 0}

                def ln_apply(tt, cc):
                    csb, b_csb = csb2[tt % 2], b_csb2[tt % 2]
                    ti = nn['n'] % 2
                    nn['n'] += 1
                    P.tt('dve', tmp[ti][:, :], csb[:, cc, :], mean_sb[:, :], ALU.subtract, r=[b_csb[cc], b_stat], w=[b_tmp[ti]])
                    P.tt('dve', tm2[ti][:, :], tmp[ti][:, :], rstd[:, :], ALU.mult, r=[b_tmp[ti], b_stat], w=[b_tm2[ti]])
                    P.act(ostg[ti][:, :], tm2[ti][:, :], AF.Silu, r=[b_tm2[ti], b_const], w=[b_ostg[ti]],
                          scale=pp[:, PP_LG + cc:PP_LG + cc + 1], bias=pp[:, PP_LB + cc:PP_LB + cc + 1])
                    P.dma('sp', MIXT[1024 + cc * 128:1024 + (cc + 1) * 128, tt * 512:(tt + 1) * 512], ostg[ti][:, :],
                          r=[b_ostg[ti]], ww=[b_out], key=("ostg", ti))

                for tt in range(NQ):
                    if tt + 1 < NQ:
                        uload(tt + 1)
                    u = tt % 2
                    csb, csq, b_csb, b_csq = csb2[u], csq2[u], b_csb2[u], b_csq2[u]
                    for cc in range(8):
                        pi = cc % 2
                        for j in range(TAPS):
                            P.mm(pc[pi][:, :], Dg[:, cc * TAPS + j, :], U[u][:, cc, j:j + 512], j == 0, j == TAPS - 1,
                                 r=[b_Dg[cc], b_U[u]], w=[b_pc[pi]])
                        cbias = pp[:, PP_CB + cc:PP_CB + cc + 1]
                        P.act(csb[:, cc, :], pc[pi][:, :], AF.Identity, r=[b_pc[pi], b_const], w=[b_csb[cc]], bias=cbias)
                        P.act(csq[:, cc, :], pc[pi][:, :], AF.Square, r=[b_pc[pi], b_const], w=[b_csq[cc]], bias=cbias)
                        P.mm(pst[0][:, :], ones_f, csb[:, cc, :], cc == 0, cc == 7, r=[b_csb[cc], b_const], w=[b_pst[0]])
                        P.mm(pst[1][:, :], ones_f, csq[:, cc, :], cc == 0, cc == 7, r=[b_csq[cc], b_const], w=[b_pst[1]])
                        if tt > 0:
                            ln_apply(tt - 1, cc)
                    P.ts('dve', mean_sb[:, :], pst[0][:, :], 1.0 / 1024, None, ALU.mult, r=[b_pst[0]], w=[b_stat])
                    P.tt('dve', msq[:, :], mean_sb[:, :], mean_sb[:, :], ALU.mult, r=[b_stat], w=[b_stat])
                    P.stt(var[:, :], pst[1][:, :], 1.0 / 1024, msq[:, :], ALU.mult, ALU.subtract, r=[b_pst[1], b_stat], w=[b_stat])
                    P.ts('dve', var[:, :], var[:, :], EPS, None, ALU.add, r=[b_stat], w=[b_stat])
                    P.act(msq[:, :], var[:, :], AF.Ln, r=[b_stat], w=[b_stat])
                    P.act(rstd[:, :], msq[:, :], AF.Exp, r=[b_stat], w=[b_stat], scale=-0.5)
                for cc in range(8):
                    ln_apply(NQ - 1, cc)
                P.emit("ph2")
            return b_out

        b_scr2 = phase2() if nph >= 2 else None

        def phase3():
            with ExitStack() as st:
                QA = [[sb(st, "QA%d_%d" % (c, i), [128, S], BF16) for i in range(2)] for c in range(2)]
                KA = [[sb(st, "KA%d_%d" % (c, i), [128, S], BF16) for i in range(2)] for c in range(2)]
                Vt = [sb(st, "Vt%d" % i, [128, NT, 128], BF16) for i in range(2)]
                NPB, NSB = 4, 3
                PT = [sb(st, "PT%d" % i, [128, 512], BF16) for i in range(NPB)]
                rr = [sb(st, "rr%d" % i, [128, 512], F32) for i in range(2)]
                tq_ = [sb(st, "tq%d" % i, [128, 512], F32) for i in range(2)]
                A = [sb(st, "A%d" % i, [128, 256], F32) for i in range(2)]
                Asq = [sb(st, "Asq%d" % i, [128, 256], F32) for i in range(2)]
                lnv = [sb(st, "lnv%d" % i, [128, 256], F32) for i in range(2)]
                ostg = [sb(st, "aostg%d" % i, [128, 256], BF16) for i in range(2)]
                sT = [ps(st, "sT%d" % i, [128, 512]) for i in range(NSB)]
                OO = [ps(st, "OO%d" % i, [128, 512]) for i in range(2)]
                LL = [ps(st, "LL%d" % i, [128, 512]) for i in range(2)]
                ssb = ps(st, "ssb", [128, 512])
                b_QA = [Buf("QA0"), Buf("QA1")]
                b_KA = [Buf("KA0"), Buf("KA1")]
                b_Vt = [Buf("Vt0"), Buf("Vt1")]
                b_PT = [Buf("PT%d" % i) for i in range(NPB)]
                b_sT = [Buf("sT%d" % i) for i in range(NSB)]
                b_OO = [Buf("OO0"), Buf("OO1")]
                b_LL = [Buf("LL0"), Buf("LL1")]
                b_ssb = Buf("ssb")
                b_rr = [Buf("rr0"), Buf("rr1")]
                b_tq = [Buf("tq0"), Buf("tq1")]
                b_A = [Buf("A0"), Buf("A1")]
                b_Asq = [Buf("Asq0"), Buf("Asq1")]
                b_lnv = [Buf("lnv0"), Buf("lnv1")]
                b_ostg = [Buf("aostg0"), Buf("aostg1")]
                b_out = Buf("scr3")
                for i in range(2):
                    P.add('pool', lambda e, i=i: e.memset(QA[1][i][0:64, :], 0.0), w=[b_QA[i]])
                    P.add('pool', lambda e, i=i: e.memset(QA[0][i][64:128, :], 0.0), w=[b_QA[i]])
                    P.add('pool', lambda e, i=i: e.memset(KA[1][i][0:64, :], 0.0), w=[b_KA[i]])
                    P.add('pool', lambda e, i=i: e.memset(KA[0][i][64:128, :], 0.0), w=[b_KA[i]])
                    P.add('pool', lambda e, i=i: e.memset(KA[0][i][64:66, :], 1.0), w=[b_KA[i]])
                    P.add('pool', lambda e, i=i: e.memset(KA[1][i][32:34, :], 1.0), w=[b_KA[i]])
                VVv = VV.ap().rearrange("(n p) d -> p n d", p=128)
                mh256 = sb(st, "mh256", [128, 256], F32)
                b_mh = Buf("mh256")
                P.add('pool', lambda e: e.memset(mh256[:, :], -0.5), w=[b_mh])

                def hload(h):
                    s_ = h % 2
                    P.dma('sp', QA[0][s_][0:64, :], QT[h, 0:64, :], r=[b_scr1], w=[b_QA[s_]], key=("QA", s_))
                    P.dma('sp', QA[1][s_][64:128, :], QT[h, 64:128, :], r=[b_scr1], ww=[b_QA[s_]], key=("QA", s_))
                    P.dma('sp', QA[0][s_][64:66, :], qaug_d[h, :, :], ww=[b_QA[s_]], key=("QA", s_))
                    P.dma('sp', QA[1][s_][32:34, :], qaug_d[h, :, :], ww=[b_QA[s_]], key=("QA", s_))
                    P.dma('sp', KA[0][s_][0:64, :], KT[h, 0:64, :], r=[b_scr1], w=[b_KA[s_]], key=("KA", s_))
                    P.dma('sp', KA[1][s_][64:128, :], KT[h, 64:128, :], r=[b_scr1], ww=[b_KA[s_]], key=("KA", s_))
                    P.dma('sp', Vt[s_][:, :, :], VVv[:, :, h * 128:(h + 1) * 128], r=[b_scr1], w=[b_Vt[s_]], key=("Vt", s_))

                NQ2 = S // 256
                blocks = []
                nqt = 0
                for h in range(H):
                    for qt in range(NQ2):
                        nkb = 2 * (qt + 1)
                        for kb in range(nkb):
                            j = kb - 2 * qt
                            blocks.append(dict(h=h, qt=qt, kb=kb, j=j, cs=(j * 128 if j >= 0 else 0), nkb=nkb,
                                               sb=len(blocks) % NSB, pb=len(blocks) % NPB, ob=nqt % 2, ep=nqt))
                        nqt += 1

                def qk(b):
                    s_, cs, q0 = b['h'] % 2, b['cs'], b['qt'] * 256
                    diag = b['j'] >= 0
                    for c in range(2):
                        P.mm(sT[b['sb']][:, c * 256 + cs:(c + 1) * 256], KA[c][s_][:, b['kb'] * 128:(b['kb'] + 1) * 128],
                             QA[c][s_][:, q0 + cs:q0 + 256], True, not diag, r=[b_KA[s_], b_QA[s_]],
                             w=[b_sT[b['sb']]] if c == 0 else [], ww=[] if c == 0 else [b_sT[b['sb']]])
                        if diag:
                            d0 = c * 256 + cs
                            P.mm(sT[b['sb']][:, d0:d0 + 128], ident_b, negtri_b, False, True, r=[b_const], ww=[b_sT[b['sb']]])

                def rest(b):
                    h, s_, cs, kb, nkb = b['h'], b['h'] % 2, b['cs'], b['kb'], b['nkb']
                    sbi, pb, ob = b['sb'], b['pb'], b['ob']
                    col = ALIB + h * ND + (2 * b['qt'] - kb + 3)
                    bias = cf[:, col:col + 1]
                    if cs == 0:
                        rngs = [(0, 512)]
                    else:
                        rngs = [(cs, 256), (256 + cs, 512)]
                    for ri, (a0, a1) in enumerate(rngs):
                        P.act(PT[pb][:, a0:a1], sT[sbi][:, a0:a1], AF.Exp, r=[b_sT[sbi], b_const],
                              w=[b_PT[pb]] if ri == 0 else [], ww=[] if ri == 0 else [b_PT[pb]], bias=bias, scale=0.125)
                    for ri, (a0, a1) in enumerate(rngs):
                        P.mm(OO[ob][:, a0:a1], Vt[s_][:, kb, :], PT[pb][:, a0:a1], kb == 0, kb == nkb - 1,
                             r=[b_Vt[s_], b_PT[pb]], w=[b_OO[ob]])
                        P.mm(LL[ob][:, a0:a1], ones_b, PT[pb][:, a0:a1], kb == 0, kb == nkb - 1,
                             r=[b_PT[pb], b_const], w=[b_LL[ob]])

                def epi1(b):
                    ob, e2 = b['ob'], b['ep'] % 2
                    P.add('dve', lambda e: e.reciprocal(rr[e2][:, :], LL[ob][:, :]), r=[b_LL[ob]], w=[b_rr[e2]])
                    P.tt('dve', tq_[e2][:, :], OO[ob][:, :], rr[e2][:, :], ALU.mult, r=[b_OO[ob], b_rr[e2]], w=[b_tq[e2]])
                    P.stt(A[e2][:, :], tq_[e2][:, 256:512], neglam, tq_[e2][:, 0:256], ALU.mult, ALU.add, r=[b_tq[e2], b_sm], w=[b_A[e2]])
                    P.tt('dve', Asq[e2][:, :], A[e2][:, :], A[e2][:, :], ALU.mult, r=[b_A[e2]], w=[b_Asq[e2]])

                def epi2a(b):
                    e2 = b['ep'] % 2
                    P.mm(ssb[:, 0:256], ones_f, Asq[e2][:, :], True, True, r=[b_Asq[e2], b_const], w=[b_ssb])
                    P.ts('dve', lnv[e2][:, :], ssb[:, 0:256], 1.0 / 128, EPS, ALU.mult, ALU.add, r=[b_ssb], w=[b_lnv[e2]])

                def epi2b(b):
                    e2, h, q0 = b['ep'] % 2, b['h'], b['qt'] * 256
                    P.act(Asq[e2][:, :], lnv[e2][:, :], AF.Ln, r=[b_lnv[e2]], w=[b_Asq[e2]])
                    P.act(lnv[e2][:, :], Asq[e2][:, :], AF.Exp, r=[b_Asq[e2]], w=[b_lnv[e2]], scale=-0.5)
                    P.stt(ostg[e2][:, :], A[e2][:, :], g08, lnv[e2][:, :], ALU.mult, ALU.mult, r=[b_A[e2], b_lnv[e2], b_sm], w=[b_ostg[e2]])
                    P.dma('sp', MIXT[h * 128:(h + 1) * 128, q0:q0 + 256], ostg[e2][:, :], r=[b_ostg[e2]], ww=[b_out], key=("aostg", e2))

                hload(0)
                if H > 1:
                    hload(1)
                wov = w_out.ap().rearrange("(k p) c -> p k c", p=128)
                for kq in range(4):
                    for ch in range(2):
                        P.dma('pool', wo[:, kq * 4:(kq + 1) * 4, ch * 1024:(ch + 1) * 1024],
                              wov[:, kq * 4:(kq + 1) * 4, ch * 1024:(ch + 1) * 1024], ww=[b_wo], key="wo")
                LOOK = NSB - 1
                pend = []
                nb = len(blocks)
                for i in range(min(LOOK, nb)):
                    qk(blocks[i])
                for i in range(nb):
                    if i + LOOK < nb:
                        qk(blocks[i + LOOK])
                    b = blocks[i]
                    rest(b)
                    for ent in pend:
                        ent[0] -= 1
                    for ent in [e_ for e_ in pend if e_[0] <= 0]:
                        pend.remove(ent)
                        if ent[1] == 'a':
                            epi2a(ent[2])
                            pend.append([3, 'b', ent[2]])
                        else:
                            epi2b(ent[2])
                    if b['kb'] == b['nkb'] - 1:
                        for ent in list(pend):
                            if ent[2]['ep'] <= b['ep'] - 2:
                                pend.remove(ent)
                                if ent[1] == 'a':
                                    epi2a(ent[2])
                                epi2b(ent[2])
                        epi1(b)
                        pend.append([12, 'a', b])
                    if b['kb'] == b['nkb'] - 1 and b['qt'] == NQ2 - 1 and b['h'] + 2 < H:
                        hload(b['h'] + 2)
                for ent in pend:
                    if ent[1] == 'a':
                        epi2a(ent[2])
                    epi2b(ent[2])
                P.emit("ph3")
            return b_out

        mid = ExitStack()
        top.enter_context(mid)
        wo = sb(mid, "wo", [128, KC, D], BF16)
        b_wo = Buf("wo")
        b_scr3 = phase3() if nph >= 3 else None

        def phase4():
            with ExitStack() as st:
                g2 = sb(st, "g2", [128, D], F32)
                brt = sb(st, "brt", [128, 36], F32)
                mT = [sb(st, "mT%d" % i, [128, KC, 512], BF16) for i in range(2)]
                xt = [sb(st, "x4_%d" % i, [128, D], F32) for i in range(2)]
                hs = [sb(st, "hs%d" % i, [128, D], F32) for i in range(2)]
                xn2 = sb(st, "xn2", [128, D], F32)
                xn2b = [sb(st, "xn2b%d" % i, [128, D], BF16) for i in range(2)]
                jk = sb(st, "jk4", [128, D], BF16)
                xT32 = sb(st, "xT32", [128, KC, 128], F32)
                rs = [sb(st, "rs%d" % i, [128, 160], F32) for i in range(2)]
                cum = [sb(st, "cum%d" % i, [128, 32], F32) for i in range(2)]
                po = [ps(st, "po%d" % i, [128, 512]) for i in range(4)]
                ptr = ps(st, "ptr", [128, 1024])
                prt = ps(st, "prt", [128, 512])
                ppre = ps(st, "ppre", [128, 512])
                b_g2 = Buf("g2")
                b_mT = [Buf("mT0"), Buf("mT1")]
                b_xt = [Buf("x40"), Buf("x41")]
                b_hs = [Buf("hs0"), Buf("hs1")]
                b_xn2, b_jk, b_xT32 = Buf("xn2"), Buf("jk4"), Buf("xT32")
                b_xn2b = [Buf("xn2b0"), Buf("xn2b1")]
                b_rs = [Buf("rs0"), Buf("rs1")]
                b_cum = [Buf("cum0"), Buf("cum1")]
                b_po = [Buf("po%d" % i) for i in range(4)]
                b_ptr, b_prt, b_ppre = Buf("ptr"), Buf("prt"), Buf("ppre")
                b_H, b_XS = Buf("HH"), Buf("XS")
                P.dma('sp', g2[:, :], bc_d[:, 0:D], w=[b_g2], key="g2")
                P.dma('sp', brt[:, :], bc_d[:, 2 * D:2 * D + 36], ww=[b_g2], key="g2")
                P.add('dve', lambda e: e.memset(cum[0][:, :], 0.0), w=[b_cum[0]])
                MIXv = MIXT.ap().rearrange("(k p) t -> p k t", p=128)

                def mload(tq):
                    P.dma('sp', mT[tq % 2][:, :, :], MIXv[:, :, tq * 512:(tq + 1) * 512], r=[b_scr2, b_scr3], w=[b_mT[tq % 2]], key=("mT", tq % 2))

                def xload(i):
                    P.dma('sp', xt[i % 2][:, :], x[i * 128:(i + 1) * 128, :], w=[b_xt[i % 2]], key=("x4", i % 2))

                def mm_tile(i):
                    tq, sub = i // 4, i % 4
                    m, bm = mT[tq % 2], b_mT[tq % 2]
                    for cbk in range(4):
                        for k in range(KC):
                            P.mm(po[cbk][:, :], m[:, k, sub * 128:(sub + 1) * 128], wo[:, k, cbk * 512:(cbk + 1) * 512], k == 0, k == KC - 1,
                                 r=[bm, b_wo], w=[b_po[cbk]])

                def adds(i):
                    h_, bh = hs[i % 2], b_hs[i % 2]
                    for cbk in range(4):
                        P.tt('dve', h_[:, cbk * 512:(cbk + 1) * 512], po[cbk][:, :], xt[i % 2][:, cbk * 512:(cbk + 1) * 512], ALU.add,
                             r=[b_po[cbk], b_xt[i % 2]], w=[bh] if cbk == 0 else [], ww=[] if cbk == 0 else [bh])
                    P.dma('sp', HH[i * 128:(i + 1) * 128, :], h_[:, :], r=[bh], ww=[b_H], key=("hs", i % 2))

                def part1(i):
                    h_, bh = hs[i % 2], b_hs[i % 2]
                    R, bR = rs[i % 2], b_rs[i % 2]
                    P.ttr(jk[:, :], h_[:, :], h_[:, :], R[:, 0:1], r=[bh], w=[b_jk, bR])
                    P.ts('dve', R[:, 1:2], R[:, 0:1], 1.0 / D, EPS, ALU.mult, ALU.add, r=[bR], w=[bR])
                    P.tt('pool', R[:, 2:3], R[:, 1:2], mhalf, ALU.pow, r=[bR, b_sm], w=[bR])
                    P.stt(xn2[:, :], h_[:, :], R[:, 2:3], g2[:, :], ALU.mult, ALU.mult, r=[bh, bR, b_g2], w=[b_xn2])
                    P.cp('act', xn2b[i % 2][:, :], xn2[:, :], r=[b_xn2], w=[b_xn2b[i % 2]])

                def part2(i):
                    R, bR = rs[i % 2], b_rs[i % 2]
                    for hb in range(2):
                        for j in range(8):
                            k = hb * 8 + j
                            P.tr(ptr[:, j * 128:(j + 1) * 128], xn2[:, k * 128:(k + 1) * 128], ident, r=[b_xn2, b_const], w=[b_ptr])
                        P.cp('act' if hb == 0 else 'dve', xT32[:, hb * 8:(hb + 1) * 8, :], ptr[:, :].rearrange("p (k t) -> p k t", t=128),
                             r=[b_ptr], w=[b_xT32] if hb == 0 else [], ww=[] if hb == 0 else [b_xT32])
                    for k in range(KC):
                        P.mm(prt[:, 0:36], xT32[:, k, :], wr[:, k * 36:(k + 1) * 36], k == 0, k == KC - 1, r=[b_xT32, b_const], w=[b_prt])
                    lg = R[:, 4:40]
                    P.tt('dve', lg, prt[:, 0:36], brt[:, :], ALU.add, r=[b_prt, b_g2], w=[bR])
                    P.add('dve', lambda e, R=R: e.reduce_max(R[:, 40:41], R[:, 4:8], AX.X), r=[bR], w=[bR])
                    P.ts('dve', R[:, 41:42], R[:, 40:41], -1.0, None, ALU.mult, r=[bR], w=[bR])
                    P.act(R[:, 44:48], R[:, 4:8], AF.Exp, r=[bR], w=[bR], bias=R[:, 41:42], accum_out=R[:, 42:43])
                    P.add('dve', lambda e, R=R: e.reciprocal(R[:, 43:44], R[:, 42:43]), r=[bR], w=[bR])
                    P.ts('dve', R[:, 48:52], R[:, 4:8], R[:, 40:41], None, ALU.is_equal, r=[bR], w=[bR])
                    goh_b = bass.AP(R, R[:, 48:52].offset, [list(R[:, 48:52].ap[0]), [1, 4], [0, 8]])
                    P.ts('dve', R[:, 52:84].rearrange("p (g e) -> p g e", e=8), goh_b, 1e30, -1e30, ALU.mult, ALU.add, r=[bR], w=[bR])
                    P.tt('dve', R[:, 84:116], R[:, 8:40], R[:, 52:84], ALU.add, r=[bR], w=[bR])
                    P.add('dve', lambda e, R=R: e.max(R[:, 116:124], R[:, 84:116]), r=[bR], w=[bR])
                    P.ts('dve', R[:, 52:84], R[:, 84:116], R[:, 116:117], None, ALU.is_equal, r=[bR], w=[bR])
                    P.ts('dve', R[:, 124:156], R[:, 84:116], R[:, 117:118], None, ALU.is_equal, r=[bR], w=[bR])
                    P.tt('dve', R[:, 156:157], R[:, 117:118], R[:, 116:117], ALU.subtract, r=[bR], w=[bR])
                    P.act(R[:, 157:158], R[:, 156:157], AF.Exp, r=[bR], w=[bR])
                    P.ts('dve', R[:, 158:159], R[:, 157:158], 1.0, None, ALU.add, r=[bR], w=[bR])
                    P.add('dve', lambda e, R=R: e.reciprocal(R[:, 159:160], R[:, 158:159]), r=[bR], w=[bR])
                    P.tt('dve', rt[:, i:i + 1], R[:, 159:160], R[:, 43:44], ALU.mult, r=[bR], w=[b_rt])
                    P.tt('dve', rt[:, NT + i:NT + i + 1], R[:, 157:158], rt[:, i:i + 1], ALU.mult, r=[bR, b_rt], w=[b_rt])
                    P.tt('dve', R[:, 84:116], R[:, 52:84], R[:, 124:156], ALU.add, r=[bR], w=[bR])

                def part3(i):
                    R, bR = rs[i % 2], b_rs[i % 2]
                    P.mm(ppre[:, 0:32], triU, R[:, 84:116], True, True, r=[bR, b_const], w=[b_ppre])
                    P.mm(ppre[:, 32:64], ones_f, R[:, 84:116], True, True, r=[bR, b_const], w=[b_ppre])
                    c0, c1 = cum[i % 2], cum[(i + 1) % 2]
                    P.tt('dve', R[:, 84:116], ppre[:, 0:32], c0[:, :], ALU.add, r=[b_ppre, b_cum[i % 2]], w=[bR])
                    P.tt('dve', c1[:, :], ppre[:, 32:64], c0[:, :], ALU.add, r=[b_ppre, b_cum[i % 2]], w=[b_cum[(i + 1) % 2]])
                    P.ts('dve', R[:, 84:116], R[:, 84:116], float(CAP - 1), None, ALU.min, r=[bR], w=[bR])
                    P.tt('dve', R[:, 84:116], R[:, 84:116], ebase, ALU.add, r=[bR, b_const], w=[bR])
                    P.stt(R[:, 4:36], R[:, 52:84], 1.0, R[:, 84:116], ALU.mult, ALU.mult, r=[bR], w=[bR], accum_out=R[:, 36:37])
                    P.stt(R[:, 4:36], R[:, 124:156], 1.0, R[:, 84:116], ALU.mult, ALU.mult, r=[bR], w=[bR], accum_out=R[:, 37:38])
                    P.cp('dve', rti[:, i:i + 1], R[:, 36:37], r=[bR], w=[b_rt])
                    P.cp('dve', rti[:, NT + i:NT + i + 1], R[:, 37:38], r=[bR], w=[b_rt])
                    P.cp('dve', rt[:, 2 * NT + i:2 * NT + i + 1], R[:, 36:37], r=[bR], w=[b_rt])
                    P.cp('dve', rt[:, 3 * NT + i:3 * NT + i + 1], R[:, 37:38], r=[bR], w=[b_rt])
                    for kk in range(2):
                        ia = rti[:, kk * NT + i:kk * NT + i + 1]
                        P.add('pool', lambda e, ia=ia, src=xn2b[i % 2]: e.indirect_dma_start(
                            out=XS[:, :], out_offset=bass.IndirectOffsetOnAxis(ap=ia, axis=0), in_=src[:, :], in_offset=None),
                            r=[b_xn2b[i % 2], b_rt], ww=[b_XS], dkey=("sc", i % 2, kk))

                mload(0)
                xload(0)
                if NQ > 1:
                    mload(1)
                mm_tile(0)
                adds(0)
                for i in range(NT):
                    tq, sub = i // 4, i % 4
                    if sub == 3 and tq + 2 < NQ:
                        mload(tq + 2)
                    if i + 1 < NT:
                        xload(i + 1)
                    part1(i)
                    if i + 1 < NT:
                        mm_tile(i + 1)
                        adds(i + 1)
                    if i >= 1:
                        part3(i - 1)
                    part2(i)
                part3(NT - 1)
                if dbg:
                    P.dma('sp', RT[:, :], rt[:, :], r=[b_rt], w=[Buf("RTd")], key="rtd")
                P.emit("ph4")
            return b_H, b_XS

        if nph >= 4:
            b_H, b_XS = phase4()
        mid.close()

        def phase5():
            with ExitStack() as st:
                NR = 32
                ring = [sb(st, "ring%d" % i, [128, 2048], BF16) for i in range(NR)]
                NXB = 2 * NST
                xsb = [sb(st, "xsb%d" % i, [128, D], BF16) for i in range(NXB)]
                xT = sb(st, "xT5", [128, KC, CAP], BF16)
                hT = sb(st, "hT5", [128, 8, CAP], BF16)
                sgt = [sb(st, "sg5_%d" % i, [128, CAP], F32) for i in range(2)]
                ystg = [sb(st, "ystg%d" % i, [128, D], F32) for i in range(2)]
                ptx = ps(st, "ptx", [128, 2048], BF16)
                pg = [ps(st, "pg%d" % i, [128, 512]) for i in range(2)]
                pu = [ps(st, "pu%d" % i, [128, 512]) for i in range(2)]
                pd = [ps(st, "pd%d" % i, [128, 512]) for i in range(2)]
                b_ring = [Buf("ring%d" % i) for i in range(NR)]
                b_xsb = [Buf("xsb%d" % i) for i in range(NXB)]
                b_xT = [Buf("xT5_%d" % i) for i in range(NST)]
                b_hT = [Buf("hT5_%d" % i) for i in range(8)]
                b_sgt = [Buf("sg50"), Buf("sg51")]
                b_ystg = [Buf("ystg0"), Buf("ystg1")]
                b_ptx = Buf("ptx")
                b_pg, b_pu, b_pd = [Buf("pg0"), Buf("pg1")], [Buf("pu0"), Buf("pu1")], [Buf("pd0"), Buf("pd1")]
                b_YS = Buf("YS")
                cnt = {'r': 0, 'x': 0, 'pd': 0, 'y': 0, 'ev': 0}

                def wloads(e):
                    sg, sd = [], []
                    for k in range(KC):
                        s_ = cnt['r'] % NR
                        cnt['r'] += 1
                        P.dma('pool', ring[s_][:, 0:1024], w_gate[e, k * 128:(k + 1) * 128, :], w=[b_ring[s_]], key=("ring", s_))
                        P.dma('pool', ring[s_][:, 1024:2048], w_up[e, k * 128:(k + 1) * 128, :], ww=[b_ring[s_]], key=("ring", s_))
                        sg.append(s_)
                    for k in range(8):
                        s_ = cnt['r'] % NR
                        cnt['r'] += 1
                        P.dma('pool', ring[s_][:, :], w_down[e, k * 128:(k + 1) * 128, :], w=[b_ring[s_]], key=("ring", s_))
                        sd.append(s_)
                    return sg, sd

                def xloads(e):
                    ids = []
                    for s3 in range(NST):
                        xi = cnt['x'] % NXB
                        cnt['x'] += 1
                        r0 = e * CAP + s3 * 128
                        P.dma('sp', xsb[xi][:, :], XS[r0:r0 + 128, :], r=[b_XS], w=[b_xsb[xi]], key=("xsb", xi))
                        ids.append(xi)
                    return ids

                nw = wloads(0)
                nx = xloads(0)
                for e in range(NE):
                    (sg, sd), xids = nw, nx
                    if e + 1 < NE:
                        nx = xloads(e + 1)
                    for s3 in range(NST):
                        xi = xids[s3]
                        for k in range(KC):
                            P.tr(ptx[:, k * 128:(k + 1) * 128], xsb[xi][:, k * 128:(k + 1) * 128], ident_b, r=[b_xsb[xi], b_const], w=[b_ptx])
                        for hb in range(2):
                            P.cp('act' if hb == 0 else 'dve', xT[:, hb * 8:(hb + 1) * 8, s3 * 128:(s3 + 1) * 128],
                                 ptx[:, hb * 1024:(hb + 1) * 1024].rearrange("p (k t) -> p k t", t=128),
                                 r=[b_ptx], w=[b_xT[s3]] if hb == 0 else [], ww=[] if hb == 0 else [b_xT[s3]])
                    for fe in range(8):
                        pi = fe % 2
                        for k in range(KC):
                            P.mm(pg[pi][:, 0:CAP], ring[sg[k]][:, fe * 128:(fe + 1) * 128], xT[:, k, :], k == 0, k == KC - 1,
                                 r=[b_ring[sg[k]]] + b_xT, w=[b_pg[pi]])
                        for k in range(KC):
                            P.mm(pu[pi][:, 0:CAP], ring[sg[k]][:, 1024 + fe * 128:1024 + (fe + 1) * 128], xT[:, k, :], k == 0, k == KC - 1,
                                 r=[b_ring[sg[k]]] + b_xT, w=[b_pu[pi]])
                        P.act(sgt[pi][:, :], pg[pi][:, 0:CAP], AF.Silu, r=[b_pg[pi]], w=[b_sgt[pi]])
                        P.tt('dve', hT[:, fe, :], sgt[pi][:, :], pu[pi][:, 0:CAP], ALU.mult, r=[b_sgt[pi], b_pu[pi]], w=[b_hT[fe]])
                    if e + 1 < NE:
                        nw = wloads(e + 1)
                    for s3 in range(NST):
                        yi = cnt['y'] % 2
                        cnt['y'] += 1
                        for cbk in range(4):
                            pi = cnt['pd'] % 2
                            cnt['pd'] += 1
                            for k in range(8):
                                P.mm(pd[pi][:, :], hT[:, k, s3 * 128:(s3 + 1) * 128], ring[sd[k]][:, cbk * 512:(cbk + 1) * 512], k == 0, k == 7,
                                     r=[b_hT[k], b_ring[sd[k]]], w=[b_pd[pi]])
                            cnt['ev'] += 1
                            P.cp('act' if cnt['ev'] % 2 else 'dve', ystg[yi][:, cbk * 512:(cbk + 1) * 512], pd[pi][:, :],
                                 r=[b_pd[pi]], w=[b_ystg[yi]] if cbk == 0 else [], ww=[] if cbk == 0 else [b_ystg[yi]])
                        r0 = e * CAP + s3 * 128
                        P.dma('sp', YS[r0:r0 + 128, :], ystg[yi][:, :], r=[b_ystg[yi]], ww=[b_YS], key=("ystg", yi))
                P.emit("ph5")
            return b_YS

        if nph >= 5:
            b_YS = phase5()

        def phase6():
            with ExitStack() as st:
                gf = sb(st, "gf", [128, D], F32)
                hb_ = [sb(st, "h6_%d" % i, [128, D], F32) for i in range(2)]
                y1 = [sb(st, "y1_%d" % i, [128, D], F32) for i in range(2)]
                y2 = [sb(st, "y2_%d" % i, [128, D], F32) for i in range(2)]
                ob = [sb(st, "ob%d" % i, [128, D], F32) for i in range(2)]
                jk = sb(st, "jk6", [128, D], BF16)
                s6 = [sb(st, "s6_%d" % i, [128, 4], F32) for i in range(2)]
                b_gf = Buf("gf")
                b_h = [Buf("h60"), Buf("h61")]
                b_y1 = [Buf("y10"), Buf("y11")]
                b_y2 = [Buf("y20"), Buf("y21")]
                b_ob = [Buf("ob0"), Buf("ob1")]
                b_jk = Buf("jk6")
                b_s6 = [Buf("s60"), Buf("s61")]
                b_fin = Buf("fin")
                P.dma('sp', gf[:, :], bc_d[:, D:2 * D], w=[b_gf], key="gf")

                def loads(i):
                    a = i % 2
                    P.dma('sp', hb_[a][:, :], HH[i * 128:(i + 1) * 128, :], r=[b_H], w=[b_h[a]], key=("h6", a))
                    for kk, (yy, by) in enumerate(((y1, b_y1), (y2, b_y2))):
                        ia = rti[:, kk * NT + i:kk * NT + i + 1]
                        P.add('pool', lambda e, ia=ia, dst=yy[a]: e.indirect_dma_start(
                            out=dst[:, :], out_offset=None, in_=YS[:, :], in_offset=bass.IndirectOffsetOnAxis(ap=ia, axis=0)),
                            r=[b_YS, b_rt], w=[by[a]], dkey=("ga", a, kk))
                loads(0)
                for i in range(NT):
                    if i + 1 < NT:
                        loads(i + 1)
                    a = i % 2
                    P.stt(hb_[a][:, :], y1[a][:, :], rt[:, i:i + 1], hb_[a][:, :], ALU.mult, ALU.add, r=[b_y1[a], b_rt], w=[b_h[a]])
                    P.stt(hb_[a][:, :], y2[a][:, :], rt[:, NT + i:NT + i + 1], hb_[a][:, :], ALU.mult, ALU.add, r=[b_y2[a], b_rt], w=[b_h[a]])
                    P.act(jk[:, :], hb_[a][:, :], AF.Square, r=[b_h[a]], w=[b_jk, b_s6[a]], accum_out=s6[a][:, 0:1])
                    P.ts('dve', s6[a][:, 1:2], s6[a][:, 0:1], 1.0 / D, EPS, ALU.mult, ALU.add, r=[b_s6[a]], w=[b_s6[a]])
                    P.tt('pool', s6[a][:, 2:3], s6[a][:, 1:2], mhalf, ALU.pow, r=[b_s6[a], b_sm], w=[b_s6[a]])
                    P.stt(ob[a][:, :], hb_[a][:, :], s6[a][:, 2:3], gf[:, :], ALU.mult, ALU.mult, r=[b_h[a], b_s6[a], b_gf], w=[b_ob[a]])
                    P.dma('sp', out_d[i * 128:(i + 1) * 128, :], ob[a][:, :], r=[b_ob[a]], ww=[b_fin], key=("ob", a))
                P.add('sp', lambda e: e.nop(), r=[b_fin])
                P.emit("ph6")

        if nph >= 6:
            phase6()
    return nc


def make_consts(S, CAP):
    NT = S // 128
    ND = NT + 3
    k = np.arange(128)
    cf = np.zeros((128, 416 + 8 * ND), np.float32)
    cf[:, 0:128] = np.eye(128, dtype=np.float32)
    cf[:, 128:256] = (k[:, None] < k[None, :]).astype(np.float32)
    cf[:, 256:384] = 1.0
    cf[:, 384:416] = (np.arange(32) * CAP)[None, :]
    sl = _slopes()
    for h in range(H):
        for di in range(ND):
            cf[:, 416 + h * ND + di] = sl[h] * (k - 128.0 * (di - 3))
    cb = np.zeros((128, 512), np.float32)
    cb[:, 384:512] = np.where(k[:, None] > k[None, :], -30000.0, 0.0)
    cb[:, 0:128] = np.eye(128)
    cb[:, 128:256] = 1.0
    cb[:, 256:384] = (k[:, None] <= k[None, :])
    cb = cb.astype(ml_dtypes.bfloat16)
    qi = np.arange(S) % 256
    lo = qi % 256
    qaug = np.stack([np.stack([-8.0 * sl[h] * lo, -8.0 * sl[h] * (qi - lo)]) for h in range(H)]).astype(np.float32).astype(ml_dtypes.bfloat16)
    return cf, cb, qaug


def make_params(inp):
    f = lambda a: np.asarray(a, np.float32)
    pp = np.zeros((128, 16 + 8 * TAPS + 24 + 1 + 256), np.float32)
    pp[:, 0:16] = f(inp['norm_mix_g'])[0].reshape(16, 128).T
    cw = f(inp['conv_w'])[0, :, 0, :]
    pp[:, 16:16 + 8 * TAPS] = cw.reshape(TAPS, 8, 128).transpose(2, 1, 0).reshape(128, 8 * TAPS)
    o = 16 + 8 * TAPS
    pp[:, o:o + 8] = f(inp['conv_b'])[0].reshape(8, 128).T
    pp[:, o + 8:o + 16] = f(inp['conv_ln_g'])[0].reshape(8, 128).T
    pp[:, o + 16:o + 24] = f(inp['conv_ln_b'])[0].reshape(8, 128).T
    pp[:, o + 24] = f(inp['subln_g'])[0]
    lam = np.concatenate([f(inp['lambda_q1'])[0], f(inp['lambda_k1'])[0], f(inp['lambda_q2'])[0], f(inp['lambda_k2'])[0]])
    pp[:, o + 25:o + 25 + 256] = lam[None, :]
    bc = np.zeros((128, 2 * D + 36), np.float32)
    bc[:, 0:D] = f(inp['norm_ffn_g'])[0][None, :]
    bc[:, D:2 * D] = f(inp['norm_final_g'])[None, :]
    bc[:, 2 * D:2 * D + 4] = f(inp['b_group_router'])[0][None, :]
    bc[:, 2 * D + 4:] = f(inp['b_expert_router'])[0][None, :]
    wrc = np.concatenate([f(inp['w_group_router'])[0], f(inp['w_expert_router'])[0]], axis=1)
    wr = np.ascontiguousarray(wrc.reshape(16, 128, 36).transpose(1, 0, 2).reshape(128, 16 * 36))
    return pp, bc, wr


def make_in_maps(inp, S, CAP, cores):
    cf, cb, qaug = make_consts(S, CAP)
    pp, bc, wr = make_params(inp)
    f = lambda a: np.ascontiguousarray(np.asarray(a, np.float32))
    shared = dict(w_in=f(inp['w_in'])[0], w_out=f(inp['w_out'])[0], w_gate=f(inp['w_gate'])[0], w_up=f(inp['w_up'])[0],
                  w_down=f(inp['w_down'])[0], cf=cf, cb=cb, qaug=qaug, pp=pp, bc=bc, wr=wr)
    xs = np.asarray(inp['x'], np.float32)
    maps = []
    for b in cores:
        m = dict(shared)
        m['x'] = np.ascontiguousarray(xs[b, :S])
        maps.append(m)
    return maps


_NC_CACHE = {}


def kernel(**inputs):
    S, CAP = 4096, 384
    if 'nc' not in _NC_CACHE:
        _NC_CACHE['nc'] = build(S, CAP)
    nc = _NC_CACHE['nc']
    maps = make_in_maps(inputs, S, CAP, list(range(8)))
    res = run_bass_kernel_spmd(nc, maps, core_ids=list(range(8)))
    return np.stack([np.asarray(r["out"], np.float32) for r in res.results], axis=0)
```
